# Optimizing a Trainium2 kernel written in Bass

```python
import math
import jax
import jax.numpy as jnp
from jax import lax
import numpy as np

D_MODEL = 2048
BATCH = 4
SEQ = 4096
DEPTH = 2

GRID_W = 64
CTX_LEN = 256
HEAD_DIM = 64
Q_BLOCK = 128
ROPE_THETA = 10000.0
EPS = 1e-6
A_HEADS = 4
A_QK_DIM = 64
A_V_DIM = 128
B_HEADS = 8
B_KV_HEADS = 2
C_HEADS = 8
C_Q_RANK = 768
C_KV_RANK = 256
C_NOPE_DIM = 64
C_ROPE_DIM = 32
C_V_DIM = 64
D_HEADS = 8
D_KV_HEADS = 2
WINDOW = 128
A_Q_COLS = 2 * A_HEADS * A_QK_DIM
B_Q_COLS = B_HEADS * HEAD_DIM
C_Q_COLS = C_Q_RANK
D_Q_COLS = D_HEADS * HEAD_DIM
A_KV_COLS = 2 * A_HEADS * A_QK_DIM + A_HEADS * A_V_DIM
B_KV_COLS = 2 * B_KV_HEADS * HEAD_DIM
C_KV_COLS = C_KV_RANK + C_ROPE_DIM
D_KV_COLS = 2 * D_KV_HEADS * HEAD_DIM
Q_SIZES = (A_Q_COLS, B_Q_COLS, C_Q_COLS, D_Q_COLS)
KV_SIZES = (A_KV_COLS, B_KV_COLS, C_KV_COLS, D_KV_COLS)
Q_COLS = A_Q_COLS + B_Q_COLS + C_Q_COLS + D_Q_COLS
KV_COLS = A_KV_COLS + B_KV_COLS + C_KV_COLS + D_KV_COLS
IN_COLS = Q_COLS + KV_COLS
MIX_WIDTH = A_HEADS * A_V_DIM + B_HEADS * HEAD_DIM + C_HEADS * C_V_DIM + D_HEADS * HEAD_DIM
D_FF = 5632
N_EXPERTS = 8
TOP_K = 2
EXPERT_FF = 5632
N_DENSE = (DEPTH + 1) // 2
N_MOE = DEPTH // 2

kernel_name = 'hybrid_parallel_heads_flow_block'


def rms_norm(x, g):
    xf = x.astype(jnp.float32)
    y = xf * lax.rsqrt(jnp.mean(xf * xf, axis=-1, keepdims=True) + EPS)
    return (y * g.astype(jnp.float32)).astype(x.dtype)


def split_cols(t, sizes):
    out = []
    o = 0
    for s in sizes:
        out.append(t[..., o:o + s])
        o += s
    return out


def split_heads(t, n, d):
    return t.reshape(t.shape[0], t.shape[1], n, d)


def merge_heads(o):
    return o.reshape(o.shape[0], o.shape[1], -1)


def rope_tables(rows, cols, rot_dim):
    axis_dim = rot_dim // 2
    inv = ROPE_THETA ** (-jnp.arange(0, axis_dim, 2, dtype=jnp.float32) / axis_dim)
    ar = rows.astype(jnp.float32)[:, None] * inv[None, :]
    ac = cols.astype(jnp.float32)[:, None] * inv[None, :]
    return (jnp.cos(ar), jnp.sin(ar), jnp.cos(ac), jnp.sin(ac))


def _rotate(x, cos, sin):
    n = x.shape[-1] // 2
    x1, x2 = x[..., :n], x[..., n:]
    return jnp.concatenate([x1 * cos - x2 * sin, x2 * cos + x1 * sin], axis=-1)


def apply_rope2d(x, tables):
    shape = (x.shape[1],) + (1,) * (x.ndim - 3) + (tables[0].shape[-1],)
    cr, sr, cc, sc = (t.reshape(shape).astype(x.dtype) for t in tables)
    half = x.shape[-1] // 2
    return jnp.concatenate([_rotate(x[..., :half], cr, sr), _rotate(x[..., half:], cc, sc)], axis=-1)


def _attend_block(q, k, v, scale, sink=None, mask=None):
    s = jnp.einsum('bqgrd,bkgd->bgrqk', q, k).astype(jnp.float32) * scale
    if mask is not None:
        s = jnp.where(mask, s, -jnp.inf)
    if sink is not None:
        g, r = q.shape[2], q.shape[3]
        sk = jnp.broadcast_to(sink.astype(jnp.float32).reshape(1, g, r, 1, 1), s.shape[:-1] + (1,))
        p = jax.nn.softmax(jnp.concatenate([s, sk], axis=-1), axis=-1)[..., :-1]
    else:
        p = jax.nn.softmax(s, axis=-1)
    return jnp.einsum('bgrqk,bkgd->bqgrd', p.astype(v.dtype), v)


def dense_attention(q, k, v, scale, sink=None):
    bsz, sq, n_heads, dk = q.shape
    g = k.shape[2]
    r = n_heads // g
    nb = sq // Q_BLOCK
    qb = q.reshape(bsz, nb, Q_BLOCK, g, r, dk).swapaxes(0, 1)
    o = lax.map(lambda qi: _attend_block(qi, k, v, scale, sink), qb)
    return o.swapaxes(0, 1).reshape(bsz, sq, n_heads, v.shape[-1])


def window_attention(q, k, v, k_ctx, v_ctx, scale, sink):
    bsz, s_len, n_heads, dk = q.shape
    g = k.shape[2]
    r = n_heads // g
    nb = s_len // Q_BLOCK
    wb = WINDOW // Q_BLOCK
    band_len = (2 * wb + 1) * Q_BLOCK

    def band(t):
        tp = jnp.pad(t, ((0, 0), (WINDOW, WINDOW), (0, 0), (0, 0)))
        tb = tp.reshape(bsz, nb + 2 * wb, Q_BLOCK, g, t.shape[-1])
        return jnp.concatenate([tb[:, j:j + nb] for j in range(2 * wb + 1)], axis=2).swapaxes(0, 1)

    blk = jnp.arange(nb)[:, None, None]
    qpos = blk * Q_BLOCK + jnp.arange(Q_BLOCK)[None, :, None]
    kpos = blk * Q_BLOCK - WINDOW + jnp.arange(band_len)[None, None, :]
    band_mask = (jnp.abs(qpos - kpos) <= WINDOW) & (kpos >= 0) & (kpos < s_len)
    mask = jnp.concatenate([band_mask, jnp.ones((nb, Q_BLOCK, k_ctx.shape[1]), dtype=bool)], axis=-1)
    qb = q.reshape(bsz, nb, Q_BLOCK, g, r, dk).swapaxes(0, 1)

    def one(args):
        qi, ki, vi, mi = args
        return _attend_block(qi, jnp.concatenate([ki, k_ctx], axis=1), jnp.concatenate([vi, v_ctx], axis=1), scale, sink, mi)

    o = lax.map(one, (qb, band(k), band(v), mask))
    return o.swapaxes(0, 1).reshape(bsz, s_len, n_heads, v.shape[-1])


def diff_attention_mixer(q_lat, kv_lat, q_ctx, kv_ctx, lam_q1, lam_k1, lam_q2, lam_k2, subln, lam_init, rope):
    nk = 2 * A_HEADS * A_QK_DIM

    def qk(t):
        return t.reshape(t.shape[0], t.shape[1], 2, A_HEADS, A_QK_DIM)

    def vals(t):
        return split_heads(t, A_HEADS, A_V_DIM)

    q = apply_rope2d(qk(q_lat), rope)
    k = apply_rope2d(qk(kv_lat[..., :nk]), rope)
    v = vals(kv_lat[..., nk:])
    k_c, v_c = qk(kv_ctx[..., :nk]), vals(kv_ctx[..., nk:])
    k_all = jnp.concatenate([k_c, k], axis=1)
    v_all = jnp.concatenate([v_c, v], axis=1)
    lam = (jnp.exp(jnp.sum(lam_q1.astype(jnp.float32) * lam_k1.astype(jnp.float32)))
           - jnp.exp(jnp.sum(lam_q2.astype(jnp.float32) * lam_k2.astype(jnp.float32))) + lam_init)
    scale = A_QK_DIM ** -0.5

    def diff(qq, kk, vv):
        a1 = dense_attention(qq[:, :, 0], kk[:, :, 0], vv, scale)
        a2 = dense_attention(qq[:, :, 1], kk[:, :, 1], vv, scale)
        o = rms_norm(a1 - lam.astype(a1.dtype) * a2, subln) * (1.0 - lam_init)
        return merge_heads(o)

    o_lat = diff(q, k_all, v_all)
    o_ctx = None if q_ctx is None else diff(qk(q_ctx), k_c, v_c)
    return o_lat, o_ctx


def qknorm_gqa_mixer(q_lat, kv_lat, q_ctx, kv_ctx, q_norm, k_norm, rope):
    nk = B_KV_HEADS * HEAD_DIM

    def qproj(t):
        return rms_norm(split_heads(t, B_HEADS, HEAD_DIM), q_norm)

    def kproj(t):
        return rms_norm(split_heads(t[..., :nk], B_KV_HEADS, HEAD_DIM), k_norm)

    def vproj(t):
        return split_heads(t[..., nk:], B_KV_HEADS, HEAD_DIM)

    q = apply_rope2d(qproj(q_lat), rope)
    k = apply_rope2d(kproj(kv_lat), rope)
    v = vproj(kv_lat)
    k_c, v_c = kproj(kv_ctx), vproj(kv_ctx)
    scale = HEAD_DIM ** -0.5
    o_lat = dense_attention(q, jnp.concatenate([k_c, k], axis=1), jnp.concatenate([v_c, v], axis=1), scale)
    o_ctx = None if q_ctx is None else merge_heads(dense_attention(qproj(q_ctx), k_c, v_c, scale))
    return merge_heads(o_lat), o_ctx


def mla_mixer(q_lat, kv_lat, q_ctx, kv_ctx, q_norm, kv_norm, w_q_up, w_kv_up, rope):
    def qproj(t):
        qf = split_heads(rms_norm(t, q_norm) @ w_q_up, C_HEADS, C_NOPE_DIM + C_ROPE_DIM)
        return qf[..., :C_NOPE_DIM], qf[..., C_NOPE_DIM:]

    def kvproj(t):
        c_kv, k_rope = t[..., :C_KV_RANK], t[..., C_KV_RANK:]
        kv = split_heads(rms_norm(c_kv, kv_norm) @ w_kv_up, C_HEADS, C_NOPE_DIM + C_V_DIM)
        return kv[..., :C_NOPE_DIM], k_rope[:, :, None, :], kv[..., C_NOPE_DIM:]

    def join(nope, rope_part):
        return jnp.concatenate([nope, jnp.broadcast_to(rope_part, nope.shape[:-1] + (C_ROPE_DIM,))], axis=-1)

    qn, qr = qproj(q_lat)
    q = join(qn, apply_rope2d(qr, rope))
    kn, kr, v = kvproj(kv_lat)
    k = join(kn, apply_rope2d(kr, rope))
    kn_c, kr_c, v_c = kvproj(kv_ctx)
    k_c = join(kn_c, kr_c)
    scale = (C_NOPE_DIM + C_ROPE_DIM) ** -0.5
    o_lat = dense_attention(q, jnp.concatenate([k_c, k], axis=1), jnp.concatenate([v_c, v], axis=1), scale)
    if q_ctx is None:
        o_ctx = None
    else:
        qn_c, qr_c = qproj(q_ctx)
        o_ctx = merge_heads(dense_attention(join(qn_c, qr_c), k_c, v_c, scale))
    return merge_heads(o_lat), o_ctx


def window_sink_mixer(q_lat, kv_lat, q_ctx, kv_ctx, sink, rope):
    nk = D_KV_HEADS * HEAD_DIM
    q = apply_rope2d(split_heads(q_lat, D_HEADS, HEAD_DIM), rope)
    k = apply_rope2d(split_heads(kv_lat[..., :nk], D_KV_HEADS, HEAD_DIM), rope)
    v = split_heads(kv_lat[..., nk:], D_KV_HEADS, HEAD_DIM)
    k_c = split_heads(kv_ctx[..., :nk], D_KV_HEADS, HEAD_DIM)
    v_c = split_heads(kv_ctx[..., nk:], D_KV_HEADS, HEAD_DIM)
    scale = HEAD_DIM ** -0.5
    o_lat = window_attention(q, k, v, k_c, v_c, scale, sink)
    o_ctx = None if q_ctx is None else merge_heads(dense_attention(split_heads(q_ctx, D_HEADS, HEAD_DIM), k_c, v_c, scale, sink))
    return merge_heads(o_lat), o_ctx


def swiglu(u, w_gate, w_up, w_down):
    return (jax.nn.silu(u @ w_gate) * (u @ w_up)) @ w_down


def moe_swiglu(u, w_router, b_router, w_gate, w_up, w_down):
    logits = (u @ w_router + b_router).astype(jnp.float32)
    top_val, top_idx = lax.top_k(logits, TOP_K)
    top_w = jax.nn.softmax(top_val, axis=-1)
    gates = jnp.sum(jax.nn.one_hot(top_idx, N_EXPERTS, dtype=jnp.float32) * top_w[..., None], axis=-2)
    out = jnp.zeros_like(u)
    for e in range(N_EXPERTS):
        out = out + gates[..., e:e + 1].astype(u.dtype) * swiglu(u, w_gate[e], w_up[e], w_down[e])
    return out


def setup_inputs(seed: int = 0) -> dict:
    key = jax.random.key(seed)
    ks = iter(jax.random.split(key, 40))

    def nrm(shape, scale):
        return jax.random.normal(next(ks), shape, jnp.float32) * scale

    def gain(shape):
        return 1.0 + nrm(shape, 0.02)

    d = D_MODEL
    return {
        'x': nrm((BATCH, SEQ, d), 1.0),
        'c': nrm((BATCH, d), 1.0),
        'ctx': nrm((BATCH, CTX_LEN, d), 1.0),
        'c_ctx': nrm((d,), 1.0),
        'w_mod': nrm((DEPTH, d, 6 * d), 0.5 * d ** -0.5),
        'b_mod': nrm((DEPTH, 6 * d), 0.02),
        'g_mix': gain((DEPTH, d)),
        'g_ffn': gain((DEPTH, d)),
        'g_final': gain((d,)),
        'w_in': nrm((DEPTH, d, IN_COLS), d ** -0.5),
        'w_out': nrm((DEPTH, MIX_WIDTH, d), MIX_WIDTH ** -0.5),
        'a_lam_q1': nrm((DEPTH, A_QK_DIM), 0.1),
        'a_lam_k1': nrm((DEPTH, A_QK_DIM), 0.1),
        'a_lam_q2': nrm((DEPTH, A_QK_DIM), 0.1),
        'a_lam_k2': nrm((DEPTH, A_QK_DIM), 0.1),
        'a_subln': gain((DEPTH, A_V_DIM)),
        'b_q_norm': gain((DEPTH, HEAD_DIM)),
        'b_k_norm': gain((DEPTH, HEAD_DIM)),
        'c_q_norm': gain((DEPTH, C_Q_RANK)),
        'c_kv_norm': gain((DEPTH, C_KV_RANK)),
        'c_w_q_up': nrm((DEPTH, C_Q_RANK, C_HEADS * (C_NOPE_DIM + C_ROPE_DIM)), C_Q_RANK ** -0.5),
        'c_w_kv_up': nrm((DEPTH, C_KV_RANK, C_HEADS * (C_NOPE_DIM + C_V_DIM)), C_KV_RANK ** -0.5),
        'd_sink': nrm((DEPTH, D_HEADS), 0.5),
        'ffn_w_gate': nrm((N_DENSE, d, D_FF), d ** -0.5),
        'ffn_w_up': nrm((N_DENSE, d, D_FF), d ** -0.5),
        'ffn_w_down': nrm((N_DENSE, D_FF, d), D_FF ** -0.5),
        'moe_w_router': nrm((N_MOE, d, N_EXPERTS), d ** -0.5),
        'moe_b_router': nrm((N_MOE, N_EXPERTS), 0.01),
        'moe_w_gate': nrm((N_MOE, N_EXPERTS, d, EXPERT_FF), d ** -0.5),
        'moe_w_up': nrm((N_MOE, N_EXPERTS, d, EXPERT_FF), d ** -0.5),
        'moe_w_down': nrm((N_MOE, N_EXPERTS, EXPERT_FF, d), EXPERT_FF ** -0.5),
    }


def reference(x, c, ctx, c_ctx, w_mod, b_mod, g_mix, g_ffn, g_final, w_in, w_out,
              a_lam_q1, a_lam_k1, a_lam_q2, a_lam_k2, a_subln, b_q_norm, b_k_norm,
              c_q_norm, c_kv_norm, c_w_q_up, c_w_kv_up, d_sink,
              ffn_w_gate, ffn_w_up, ffn_w_down,
              moe_w_router, moe_b_router, moe_w_gate, moe_w_up, moe_w_down):
    n_tok = x.shape[1]
    n_rows = n_tok // GRID_W
    t = jnp.arange(n_rows * GRID_W)
    rows, cols = t // GRID_W, t % GRID_W
    rope_head = rope_tables(rows, cols, HEAD_DIM)
    rope_mla = rope_tables(rows, cols, C_ROPE_DIM)

    def ffn(l, u):
        i = l // 2
        if l % 2 == 0:
            return swiglu(u, ffn_w_gate[i], ffn_w_up[i], ffn_w_down[i])
        return moe_swiglu(u, moe_w_router[i], moe_b_router[i], moe_w_gate[i], moe_w_up[i], moe_w_down[i])

    h_lat = x
    h_ctx = ctx
    for l in range(DEPTH):
        last = l == DEPTH - 1
        lam_init = 0.8 - 0.6 * math.exp(-0.3 * l)
        n_ctx_mod = 2 * D_MODEL if last else 6 * D_MODEL
        m_lat = (jax.nn.silu(c) @ w_mod[l] + b_mod[l])[:, None, :]
        m_ctx = (jax.nn.silu(c_ctx) @ w_mod[l][:, :n_ctx_mod] + b_mod[l][:n_ctx_mod])[None, None, :]
        sh1, sc1, gt1, sh2, sc2, gt2 = jnp.split(m_lat, 6, axis=-1)
        cm = jnp.split(m_ctx, n_ctx_mod // D_MODEL, axis=-1)

        u_lat = rms_norm(h_lat, g_mix[l]) * (1 + sc1) + sh1
        u_ctx = rms_norm(h_ctx, g_mix[l]) * (1 + cm[1]) + cm[0]
        p_lat = u_lat @ w_in[l]
        qs_lat = split_cols(p_lat[..., :Q_COLS], Q_SIZES)
        kvs_lat = split_cols(p_lat[..., Q_COLS:], KV_SIZES)
        if last:
            qs_ctx = [None, None, None, None]
            kvs_ctx = split_cols(u_ctx @ w_in[l][:, Q_COLS:], KV_SIZES)
        else:
            p_ctx = u_ctx @ w_in[l]
            qs_ctx = split_cols(p_ctx[..., :Q_COLS], Q_SIZES)
            kvs_ctx = split_cols(p_ctx[..., Q_COLS:], KV_SIZES)

        oa, oa_c = diff_attention_mixer(qs_lat[0], kvs_lat[0], qs_ctx[0], kvs_ctx[0],
                                        a_lam_q1[l], a_lam_k1[l], a_lam_q2[l], a_lam_k2[l], a_subln[l], lam_init, rope_head)
        ob, ob_c = qknorm_gqa_mixer(qs_lat[1], kvs_lat[1], qs_ctx[1], kvs_ctx[1], b_q_norm[l], b_k_norm[l], rope_head)
        oc, oc_c = mla_mixer(qs_lat[2], kvs_lat[2], qs_ctx[2], kvs_ctx[2],
                             c_q_norm[l], c_kv_norm[l], c_w_q_up[l], c_w_kv_up[l], rope_mla)
        od, od_c = window_sink_mixer(qs_lat[3], kvs_lat[3], qs_ctx[3], kvs_ctx[3], d_sink[l], rope_head)

        h_lat = h_lat + gt1 * (jnp.concatenate([oa, ob, oc, od], axis=-1) @ w_out[l])
        if not last:
            h_ctx = h_ctx + cm[2] * (jnp.concatenate([oa_c, ob_c, oc_c, od_c], axis=-1) @ w_out[l])

        h_lat = h_lat + gt2 * ffn(l, rms_norm(h_lat, g_ffn[l]) * (1 + sc2) + sh2)
        if not last:
            h_ctx = h_ctx + cm[5] * ffn(l, rms_norm(h_ctx, g_ffn[l]) * (1 + cm[4]) + cm[3])

    return rms_norm(h_lat, g_final)
```

```python
import math
import numpy as np
from contextlib import ExitStack
import concourse.bass as bass
import concourse.mybir as mybir
from concourse.bass_utils import run_bass_kernel_spmd

F32 = mybir.dt.float32
BF16 = mybir.dt.bfloat16
AF = mybir.ActivationFunctionType
ALU = mybir.AluOpType
AX = mybir.AxisListType

D = 2048
KC = 16
GRID_W = 64
EPS = 1e-6
DFF = 5632
FC = 44
NEXP = 8
Q_COLS = 2304
IN_COLS = 4128
SEM_ROT = 20000
DEBUG = False
DBG = {}


class Res:
    __slots__ = ("name", "wdeps", "rdeps", "dsem", "dcnt")

    def __init__(self, name):
        self.name = name
        self.wdeps = []
        self.rdeps = []
        self.dsem = None
        self.dcnt = 0


class Sched:
    ENGS = ("pe", "act", "dve", "pool", "sp")

    def __init__(self, nc, stack):
        self.nc = nc
        self.stack = stack
        self.prog = {e: [] for e in self.ENGS}
        self.sem = {}
        self.cnt = {}
        self.water = {e: {} for e in self.ENGS}
        self.nsem = 0
        self.allsems = {}
        for e in ("pe", "act", "dve", "pool"):
            self._newsem(e)

    def _alloc_sem(self, name):
        self.nsem += 1
        return self.stack.enter_context(self.nc.semaphore(f"{name}{self.nsem}"))

    def _newsem(self, e):
        self.sem[e] = self._alloc_sem("s" + e)
        self.cnt[e] = 0

    def _note(self, tok):
        self.allsems[id(tok[0])] = tok

    def _waits(self, e, deps, skip_own):
        w = self.water[e]
        for (sem, val, owner) in deps:
            if skip_own and owner == e:
                continue
            k = id(sem)
            if w.get(k, 0) >= val:
                continue
            w[k] = val
            self.prog[e].append(("wait", sem, val))

    @staticmethod
    def _prune(deps):
        best = {}
        for d in deps:
            k = id(d[0])
            if k not in best or best[k][1] < d[1]:
                best[k] = d
        return list(best.values())

    def op(self, e, fn, reads=(), writes=()):
        deps = []
        for r in reads:
            deps += r.wdeps
        for w_ in writes:
            deps += w_.wdeps
            deps += w_.rdeps
        self._waits(e, deps, e == "pe")
        if self.cnt[e] >= SEM_ROT:
            self._newsem(e)
        self.cnt[e] += 1
        tok = (self.sem[e], self.cnt[e], e)
        self._note(tok)
        self.prog[e].append(("op", fn, self.sem[e]))
        for r in reads:
            r.rdeps.append(tok)
            if len(r.rdeps) > 16:
                r.rdeps = self._prune(r.rdeps)
        for w_ in writes:
            w_.wdeps = [tok]
            w_.rdeps = []
        return tok

    def dma(self, q, out_ap, in_ap, reads=(), writes=()):
        deps = []
        for r in reads:
            deps += r.wdeps
        for w_ in writes:
            deps += w_.wdeps
            deps += w_.rdeps
        self._waits(q, deps, False)
        tgt = writes[0]
        if tgt.dsem is None or tgt.dcnt >= SEM_ROT:
            tgt.dsem = self._alloc_sem("d")
            tgt.dcnt = 0
        tgt.dcnt += 16
        tok = (tgt.dsem, tgt.dcnt, "dma")
        self._note(tok)
        self.prog[q].append(("dma", out_ap, in_ap, tgt.dsem))
        for r in reads:
            r.rdeps.append(tok)
            if len(r.rdeps) > 16:
                r.rdeps = self._prune(r.rdeps)
        for w_ in writes:
            w_.wdeps = self._prune([d for d in w_.wdeps if d[2] == "dma"] + [tok])
            w_.rdeps = []
        return tok

    def barrier(self):
        deps = list(self.allsems.values())
        for e in self.ENGS:
            self._waits(e, deps, False)

    def emit(self):
        self.barrier()
        nc = self.nc
        prog = self.prog

        def run(eng, lst):
            for it in lst:
                if it[0] == "wait":
                    eng.wait_ge(it[1], it[2])
                elif it[0] == "op":
                    it[1](eng).then_inc(it[2], 1)
                else:
                    eng.dma_start(out=it[1], in_=it[2]).then_inc(it[3], 16)

        with nc.Block() as block:
            @block.tensor
            def _(eng):
                run(eng, prog["pe"])

            @block.scalar
            def _(eng):
                run(eng, prog["act"])

            @block.vector
            def _(eng):
                run(eng, prog["dve"])

            @block.gpsimd
            def _(eng):
                run(eng, prog["pool"])

            @block.sync
            def _(eng):
                run(eng, prog["sp"])


class Ctx:
    def __init__(self):
        self.nc = bass.Bass("TRN2", target_bir_lowering=False)
        self.st = ExitStack()
        self.S = Sched(self.nc, self.st)
        self.n = 0

    def ext_in(self, name, shape, dt=F32):
        return self.nc.dram_tensor(name, list(shape), dt, kind="ExternalInput").ap(), Res(name)

    def ext_out(self, name, shape, dt=F32):
        return self.nc.dram_tensor(name, list(shape), dt, kind="ExternalOutput").ap(), Res(name)

    def dram(self, name, shape, dt):
        kind = "ExternalOutput" if DEBUG else "Internal"
        return self.nc.dram_tensor(name, list(shape), dt, kind=kind).ap(), Res(name)

    def sb(self, name, shape, dt, stack=None):
        t = (stack or self.st).enter_context(self.nc.sbuf_tensor(name, list(shape), dt))
        return t, Res(name)

    def ps(self, name, shape, dt, stack=None):
        t = (stack or self.st).enter_context(self.nc.psum_tensor(name, list(shape), dt))
        return t, Res(name)


class Rot:
    def __init__(self, items):
        self.items = items
        self.i = 0

    def next(self):
        it = self.items[self.i % len(self.items)]
        self.i += 1
        return it


def _rstd_ops(S, ss, rstd, r_ss, r_rstd, n, inv_n):
    S.op("dve", lambda e: e.tensor_scalar(rstd[:n], ss[:n], inv_n, EPS, ALU.mult, ALU.add), reads=[r_ss], writes=[r_rstd])
    S.op("act", lambda e: e.activation(rstd[:n], rstd[:n], AF.Sqrt), reads=[r_rstd], writes=[r_rstd])
    S.op("dve", lambda e: e.reciprocal(rstd[:n], rstd[:n]), reads=[r_rstd], writes=[r_rstd])


def build_mod(ncols):
    C = Ctx()
    S = C.S
    w, r_w = C.ext_in("w", [2, D, ncols])
    b, r_b = C.ext_in("b", [2, 5, ncols])
    ct, r_ct = C.ext_in("ct", [128, KC, 5])
    outs = [C.ext_out(f"out{l}", [5, ncols]) for l in range(2)]
    cs, r_cs = C.sb("cs", [128, KC, 5], F32)
    sg, r_sg = C.sb("sg", [128, KC, 5], F32)
    chi, r_chi = C.sb("chi", [128, KC, 8], BF16)
    clo, r_clo = C.sb("clo", [128, KC, 8], BF16)
    S.dma("sp", cs[:], ct, reads=[r_ct], writes=[r_cs])
    S.op("act", lambda e: e.activation(sg[:], cs[:], AF.Sigmoid), reads=[r_cs], writes=[r_sg])
    S.op("dve", lambda e: e.tensor_tensor(cs[:], cs[:], sg[:], ALU.mult), reads=[r_cs, r_sg], writes=[r_cs])
    S.op("dve", lambda e: e.tensor_copy(chi[:, :, 0:5], cs[:]), reads=[r_cs], writes=[r_chi])
    S.op("dve", lambda e: e.tensor_copy(sg[:], chi[:, :, 0:5]), reads=[r_chi], writes=[r_sg])
    S.op("dve", lambda e: e.tensor_tensor(sg[:], cs[:], sg[:], ALU.subtract), reads=[r_cs, r_sg], writes=[r_sg])
    S.op("dve", lambda e: e.tensor_copy(clo[:, :, 0:5], sg[:]), reads=[r_sg], writes=[r_clo])
    nb = (ncols + 511) // 512
    NQ_ = KC // 4
    wf_rot = Rot([C.sb(f"wf{i}", [128, NQ_, ncols], F32) for i in range(2)])
    wr_rot = Rot([C.sb(f"wr{i}", [128, NQ_, ncols], F32) for i in range(1)])
    whi_rot = Rot([C.sb(f"whi{i}", [128, NQ_, ncols], BF16) for i in range(2)])
    wlo_rot = Rot([C.sb(f"wlo{i}", [128, NQ_, ncols], BF16) for i in range(2)])
    for l in range(2):
        bt, r_bt = C.sb(f"bt{l}", [5, ncols], F32)
        ot, r_ot = C.sb(f"ot{l}", [5, ncols], F32)
        pss = [C.ps(f"mps{l}_{i}", [128, 512], F32) for i in range(nb)]
        S.dma("sp", bt[:], b[l], reads=[r_b], writes=[r_bt])
        for q4 in range(4):
            wf, r_wf = wf_rot.next()
            S.dma("sp", wf[:], w[l, q4 * 512:(q4 + 1) * 512, :].rearrange("(c p) n -> p c n", p=128), reads=[r_w], writes=[r_wf])
            whi, r_whi = whi_rot.next()
            wlo, r_wlo = wlo_rot.next()
            wr_, r_wr = wr_rot.next()
            S.op("act", lambda e, whi=whi, wf=wf: e.copy(whi[:], wf[:]), reads=[r_wf], writes=[r_whi])
            S.op("pool", lambda e, wr_=wr_, whi=whi: e.tensor_copy(wr_[:], whi[:]), reads=[r_whi], writes=[r_wr])
            S.op("dve", lambda e, wr_=wr_, wf=wf: e.tensor_tensor(wr_[:], wf[:], wr_[:], ALU.subtract), reads=[r_wf, r_wr], writes=[r_wr])
            S.op("act", lambda e, wlo=wlo, wr_=wr_: e.copy(wlo[:], wr_[:]), reads=[r_wr], writes=[r_wlo])
            for i in range(nb):
                c0 = i * 512
                cw = min(512, ncols - c0)
                p_, r_p = pss[i]
                for kc in range(NQ_):
                    k = q4 * NQ_ + kc
                    for pi, (ca, wa, r1, r2) in enumerate(((chi, whi, r_chi, r_whi), (chi, wlo, r_chi, r_wlo), (clo, whi, r_clo, r_whi))):
                        S.op("pe", lambda e, p_=p_, k=k, kc=kc, c0=c0, cw=cw, ca=ca, wa=wa, pi=pi: e.matmul(
                            p_[0:5, 0:cw], ca[:, k, 0:5], wa[:, kc, c0:c0 + cw], start=(k == 0 and pi == 0), stop=(k == KC - 1 and pi == 2)),
                            reads=[r1, r2], writes=[r_p])
        for i in range(nb):
            c0 = i * 512
            cw = min(512, ncols - c0)
            p_, r_p = pss[i]
            S.op("dve", lambda e, p_=p_, c0=c0, cw=cw, ot=ot, bt=bt: e.tensor_tensor(ot[:, c0:c0 + cw], p_[0:5, 0:cw], bt[:, c0:c0 + cw], ALU.add),
                 reads=[r_p, r_bt], writes=[r_ot])
        S.dma("sp", outs[l][0], ot[:], reads=[r_ot], writes=[outs[l][1]])
    S.emit()
    C.st.close()
    return C.nc


class FFN:
    def __init__(self, C, T, wg, wu, wd, r_w, st=None):
        self.C, self.T = C, T
        self.wg, self.wu, self.wd, self.r_w = wg, wu, wd, r_w
        self.HT, self.r_HT = C.sb("ffn_HT", [128, FC, T], BF16, st)
        self.gw = Rot([C.sb(f"ffn_gw{i}", [128, KC, 256], BF16, st) for i in range(2)])
        self.uw = Rot([C.sb(f"ffn_uw{i}", [128, KC, 256], BF16, st) for i in range(2)])
        self.dw = Rot([C.sb(f"ffn_dw{i}", [128, FC, 128], BF16, st) for i in range(2)])
        self.sg = Rot([C.sb(f"ffn_sg{i}", [128, 512], F32, st) for i in range(2)])
        self.gps = Rot([C.ps(f"ffn_gps{i}", [128, 512], F32, st) for i in range(2)])
        self.ups = Rot([C.ps(f"ffn_ups{i}", [128, 512], F32, st) for i in range(2)])
        self.yps = Rot([C.ps(f"ffn_yps{i}", [128, 512], F32, st) for i in range(2)])

    def run(self, u2T, r_u2T, T, sink):
        C, S = self.C, self.C.S
        HT, r_HT = self.HT, self.r_HT
        ntb = (T + 511) // 512
        for g in range(FC // 2):
            gw, r_gw = self.gw.next()
            uw, r_uw = self.uw.next()
            S.dma("pool", gw[:], self.wg[:, g * 256:(g + 1) * 256].rearrange("(c p) n -> p c n", p=128), reads=[self.r_w], writes=[r_gw])
            S.dma("pool", uw[:], self.wu[:, g * 256:(g + 1) * 256].rearrange("(c p) n -> p c n", p=128), reads=[self.r_w], writes=[r_uw])
            for j in range(2):
                fc = g * 2 + j
                for tb in range(ntb):
                    t0 = tb * 512
                    tw = min(512, T - t0)
                    gp, r_gp = self.gps.next()
                    up, r_up = self.ups.next()
                    for kc in range(KC):
                        S.op("pe", lambda e, gp=gp, gw=gw, j=j, kc=kc, t0=t0, tw=tw: e.matmul(
                            gp[:, 0:tw], gw[:, kc, j * 128:(j + 1) * 128], u2T[:, kc, t0:t0 + tw], start=(kc == 0), stop=(kc == KC - 1)),
                            reads=[r_gw, r_u2T], writes=[r_gp])
                    for kc in range(KC):
                        S.op("pe", lambda e, up=up, uw=uw, j=j, kc=kc, t0=t0, tw=tw: e.matmul(
                            up[:, 0:tw], uw[:, kc, j * 128:(j + 1) * 128], u2T[:, kc, t0:t0 + tw], start=(kc == 0), stop=(kc == KC - 1)),
                            reads=[r_uw, r_u2T], writes=[r_up])
                    sg, r_sg = self.sg.next()
                    S.op("act", lambda e, sg=sg, gp=gp, tw=tw: e.activation(sg[:, 0:tw], gp[:, 0:tw], AF.Sigmoid), reads=[r_gp], writes=[r_sg])
                    S.op("dve", lambda e, sg=sg, gp=gp, tw=tw: e.tensor_tensor(sg[:, 0:tw], sg[:, 0:tw], gp[:, 0:tw], ALU.mult), reads=[r_sg, r_gp], writes=[r_sg])
                    S.op("dve", lambda e, sg=sg, up=up, fc=fc, t0=t0, tw=tw: e.tensor_tensor(HT[:, fc, t0:t0 + tw], sg[:, 0:tw], up[:, 0:tw], ALU.mult),
                         reads=[r_sg, r_up], writes=[r_HT])
        for cc in range(KC):
            dw, r_dw = self.dw.next()
            S.dma("pool", dw[:], self.wd[:, cc * 128:(cc + 1) * 128].rearrange("(c p) n -> p c n", p=128), reads=[self.r_w], writes=[r_dw])
            for tb in range(ntb):
                t0 = tb * 512
                tw = min(512, T - t0)
                yp, r_yp = self.yps.next()
                for fc in range(FC):
                    S.op("pe", lambda e, yp=yp, dw=dw, fc=fc, t0=t0, tw=tw: e.matmul(
                        yp[:, 0:tw], dw[:, fc, :], HT[:, fc, t0:t0 + tw], start=(fc == 0), stop=(fc == FC - 1)),
                        reads=[r_dw, r_HT], writes=[r_yp])
                sink(cc, tb, yp, r_yp, tw)


def build_layer(last, moe, n_own, n_oth, n_ctx, lam_init):
    C = Ctx()
    S = C.S
    nc = C.nc
    NLAT = n_own + n_oth
    NK = n_ctx + NLAT
    NKT = NK // 128
    CT = n_ctx // 128
    OT = n_own // 128
    nq_ctx = 0 if last else n_ctx
    NQ = n_own + nq_ctx
    NQB = n_own // 512

    x_own, r_in = C.ext_in("x_own", [n_own, D])
    x_oth, _ = C.ext_in("x_oth", [n_oth, D])
    x_ctx, _ = C.ext_in("x_ctx", [n_ctx, D])
    w_in, _ = C.ext_in("w_in", [D, IN_COLS])
    w_out, _ = C.ext_in("w_out", [D, D])
    wqup, _ = C.ext_in("wqup", [768, 768])
    wkvup, _ = C.ext_in("wkvup", [256, 1024])
    vpc, _ = C.ext_in("vpc", [128, 12, KC])
    vbc, _ = C.ext_in("vbc", [128, 7, D])
    rope, _ = C.ext_in("rope", [NLAT, 144])
    small, _ = C.ext_in("small", [128, 1304])
    ident_in, _ = C.ext_in("ident", [128, 128])
    masks, _ = C.ext_in("masks", [8, 128, 512])
    if not moe:
        wg, _ = C.ext_in("wg", [D, DFF])
        wu, _ = C.ext_in("wu", [D, DFF])
        wd, _ = C.ext_in("wd", [DFF, D])
        h_out, r_hout = C.ext_out("h_out", [n_own, D])
        if not last:
            hctx_out, r_hctx = C.ext_out("hctx_out", [n_ctx, D])
    else:
        wr, _ = C.ext_in("wr", [128, NEXP, D])
        hmid_out, r_hmid = C.ext_out("hmid", [n_own, D])
        u2t_out, r_u2t = C.ext_out("u2t", [D, n_own], BF16)
        gates_out, r_gates = C.ext_out("gates", [n_own, NEXP])

    NQCH = 8 + 8 + 8 + 8
    NKCH = 4 + 1 + 8 + 1
    VW = 512 + 2 * 66 + 8 * 66 + 2 * 66
    qs, r_qs = C.dram("qs", [NQCH, 128, NQ], BF16)
    ks, r_ks = C.dram("ks", [NKCH, 128, NK], BF16)
    vs, r_vs = C.dram("vs", [128, NKT, VW], BF16)
    mixT, r_mix = C.dram("mixT", [D, NQ], BF16)

    ident_f, r_idf = C.sb("ident_f", [128, 128], F32)
    ident_b, r_idb = C.sb("ident_b", [128, 128], BF16)
    S.dma("sp", ident_f[:], ident_in, reads=[r_in], writes=[r_idf])
    S.dma("pool", ident_b[:], ident_in, reads=[r_in], writes=[r_idb])
    vp, r_vp = C.sb("vp", [128, 12, KC], F32)
    S.dma("sp", vp[:], vpc, reads=[r_in], writes=[r_vp])
    sm, r_sm = C.sb("sm", [128, 1304], F32)
    S.dma("sp", sm[:], small, reads=[r_in], writes=[r_sm])
    ab, r_ab = C.sb("ab", [128, 8, KC], F32)

    def mk_ab(dst, g, sc, sh):
        S.op("dve", lambda e: e.tensor_scalar(ab[:, dst, :], vp[:, sc, :], 1.0, None, ALU.add), reads=[r_vp], writes=[r_ab])
        S.op("dve", lambda e: e.tensor_tensor(ab[:, dst, :], ab[:, dst, :], vp[:, g, :], ALU.mult), reads=[r_ab, r_vp], writes=[r_ab])
        S.op("dve", lambda e: e.tensor_copy(ab[:, dst + 1, :], vp[:, sh, :]), reads=[r_vp], writes=[r_ab])

    mk_ab(0, 0, 1, 2)
    mk_ab(2, 0, 6, 7)
    mk_ab(4, 3, 4, 5)
    mk_ab(6, 3, 8, 9)
    if DEBUG:
        abd, r_abd = C.dram("abdbg", [128, 8, KC], F32)
        S.dma("sp", abd, ab[:], reads=[r_ab], writes=[r_abd])
        vpd, r_vpd = C.dram("vpdbg", [128, 12, KC], F32)
        S.dma("sp", vpd, vp[:], reads=[r_vp], writes=[r_vpd])
    lamt, r_lam = C.sb("lamt", [128, 8], F32)
    ltmp, r_ltmp = C.sb("ltmp", [128, 64], F32)
    for i in range(2):
        S.op("dve", lambda e, i=i: e.tensor_tensor(ltmp[:], sm[:, i * 128:i * 128 + 64], sm[:, i * 128 + 64:i * 128 + 128], ALU.mult), reads=[r_sm], writes=[r_ltmp])
        S.op("dve", lambda e, i=i: e.tensor_reduce(lamt[:, i:i + 1], ltmp[:], AX.X, ALU.add), reads=[r_ltmp], writes=[r_lam])
    S.op("act", lambda e: e.activation(lamt[:, 2:4], lamt[:, 0:2], AF.Exp), reads=[r_lam], writes=[r_lam])
    S.op("dve", lambda e: e.tensor_scalar(lamt[:, 4:5], lamt[:, 3:4], -lam_init, None, ALU.add), reads=[r_lam], writes=[r_lam])
    S.op("dve", lambda e: e.tensor_tensor(lamt[:, 5:6], lamt[:, 4:5], lamt[:, 2:3], ALU.subtract), reads=[r_lam], writes=[r_lam])
    NEG_LAM = lamt[:, 5:6]
    sinkexp, r_sink = C.sb("sinkexp", [128, 8], F32)
    S.op("act", lambda e: e.activation(sinkexp[:], sm[:, 1280:1288], AF.Exp), reads=[r_sm], writes=[r_sink])
    sub_in, _ = C.ext_in("subln_pc", [128, 1])
    ckvn_in, _ = C.ext_in("ckvn", [128, 256])
    subg, r_subg = C.sb("subg", [128, 1], F32)
    S.dma("sp", subg[:], sub_in, reads=[r_in], writes=[r_subg])
    S.op("dve", lambda e: e.tensor_scalar(subg[:], subg[:], 1.0 - lam_init, None, ALU.mult), reads=[r_subg], writes=[r_subg])
    ckvn, r_ckvn = C.sb("ckvn_sb", [128, 256], F32)
    S.dma("sp", ckvn[:], ckvn_in, reads=[r_in], writes=[r_ckvn])
    ones_b, r_ones = C.sb("ones_b", [128, 128], BF16)
    S.op("pool", lambda e: e.memset(ones_b[:], 1.0), writes=[r_ones])
    ones_f, r_onesf = C.sb("ones_f", [128, 128], F32)
    S.op("pool", lambda e: e.memset(ones_f[:], 1.0), writes=[r_onesf])

    BQN = sm[:, 384:448]
    BKN = sm[:, 448:512]
    CQN = sm[:, 512:1280]

    with ExitStack() as ph:
        xt_rot = Rot([C.sb(f"xt{i}", [128, D], F32, ph) for i in range(1)])
        sq, r_sq = C.sb("sq", [128, D], BF16, ph)
        xn_rot = Rot([C.sb(f"xn{i}", [128, D], BF16, ph) for i in range(1)])
        ss_rot = Rot([C.sb(f"ss{i}", [128, 2], F32, ph) for i in range(2)])
        uT, r_uT = C.sb("uT", [128, KC, 512], BF16, ph)
        wt_rot = Rot([C.sb(f"wt{i}", [128, KC, 512], BF16, ph) for i in range(2)])
        tp_rot = Rot([C.ps(f"tp{i}", [128, 512], F32, ph) for i in range(2)])
        pj_rot = Rot([C.ps(f"pj{i}", [128, 512], F32, ph) for i in range(4)])
        rp_rot = Rot([C.sb(f"rp{i}", [128, 144], F32, ph) for i in range(2)])
        NT4 = 4
        res_tiles = [Res(f"tile{i}") for i in range(NT4)]
        praw = [C.sb(f"praw{i}", [128, 768], F32, ph)[0] for i in range(NT4)]
        t1 = [C.sb(f"t1_{i}", [128, 768], F32, ph)[0] for i in range(NT4)]
        t2 = [C.sb(f"t2_{i}", [128, 544], F32, ph)[0] for i in range(NT4)]
        qpad = [C.sb(f"qpad{i}", [128, 8, 128], BF16, ph)[0] for i in range(NT4)]
        kbf = [C.sb(f"kbf{i}", [128, 768], BF16, ph)[0] for i in range(NT4)]
        cqn = kbf
        cqT = [C.sb(f"cqT{i}", [128, 6, 128], BF16, ph)[0] for i in range(NT4)]
        ckT = cqT
        c96 = [C.sb(f"c96_{i}", [128, 8, 96], BF16, ph)[0] for i in range(NT4)]
        krope = [C.sb(f"krope{i}", [128, 32], F32, ph)[0] for i in range(NT4)]
        vrow = [C.sb(f"vrow{i}", [128, VW], BF16, ph)[0] for i in range(NT4)]
        st2 = [C.sb(f"st2_{i}", [128, 16], F32, ph)[0] for i in range(NT4)]
        qstage, r_qst = C.sb("qstage", [128, NQCH, 512], BF16, ph)
        kstage, r_kst = C.sb("kstage", [128, NKCH, 512], BF16, ph)
        wq_sb, r_wq = C.sb("wq_sb", [128, 6, 768], BF16, ph)
        wkv_sb, r_wkv = C.sb("wkv_sb", [128, 2, 1024], BF16, ph)
        S.dma("pool", wq_sb[:], wqup.rearrange("(c p) n -> p c n", p=128), reads=[r_in], writes=[r_wq])
        S.dma("pool", wkv_sb[:], wkvup.rearrange("(c p) n -> p c n", p=128), reads=[r_in], writes=[r_wkv])
        for i in range(NT4):
            S.op("pool", lambda e, i=i: e.memset(qpad[i][:], 0.0), writes=[res_tiles[i]])
            S.op("pool", lambda e, i=i: e.memset(vrow[i][:], 1.0), writes=[res_tiles[i]])

        QBLK = [("AQ", 0, 512), ("BQ", 512, 512), ("CQ1", 1024, 512), ("CQ2", 1536, 256), ("DQ", 1792, 512)]
        KVBLK = [("AK", 2304, 512), ("AV", 2816, 512), ("BKV", 3328, 256), ("CKV", 3584, 288), ("DKV", 3872, 256)]

        def rope64(i, src, nh, dst, lat, rp):
            rt = res_tiles[i]
            if not lat:
                S.op("dve", lambda e: e.tensor_copy(dst, src), reads=[rt], writes=[rt])
                return
            rpt, r_rp = rp
            a = t1[i][:, 0:nh * 64]
            b_ = t2[i][:, 0:nh * 64]
            cos = rpt[:, 0:64].unsqueeze(1).broadcast_to([128, nh, 64])
            sn = rpt[:, 64:96].rearrange("p (h s) -> p h s", h=2).unsqueeze(1).broadcast_to([128, nh, 2, 16])
            s3 = src.rearrange("p (n d) -> p n d", n=nh)
            a3 = a.rearrange("p (n d) -> p n d", n=nh)
            S.op("dve", lambda e: e.tensor_tensor(a3, s3, cos, ALU.mult), reads=[rt, r_rp], writes=[rt])
            s5 = src.rearrange("p (n h t s) -> p n h t s", n=nh, h=2, t=2)
            b5 = b_.rearrange("p (n h t s) -> p n h t s", n=nh, h=2, t=2)
            a5 = a.rearrange("p (n h t s) -> p n h t s", n=nh, h=2, t=2)
            d5 = dst.rearrange("p (n h t s) -> p n h t s", n=nh, h=2, t=2)
            S.op("dve", lambda e: e.tensor_tensor(b5[:, :, :, 0, :], s5[:, :, :, 1, :], sn, ALU.mult), reads=[rt, r_rp], writes=[rt])
            S.op("dve", lambda e: e.tensor_tensor(b5[:, :, :, 1, :], s5[:, :, :, 0, :], sn, ALU.mult), reads=[rt, r_rp], writes=[rt])
            S.op("dve", lambda e: e.tensor_tensor(d5[:, :, :, 0, :], a5[:, :, :, 0, :], b5[:, :, :, 0, :], ALU.subtract), reads=[rt], writes=[rt])
            S.op("dve", lambda e: e.tensor_tensor(d5[:, :, :, 1, :], a5[:, :, :, 1, :], b5[:, :, :, 1, :], ALU.add), reads=[rt], writes=[rt])

        def rope32(i, src3, dst3, nh, lat, rp):
            rt = res_tiles[i]
            if not lat:
                S.op("dve", lambda e: e.tensor_copy(dst3, src3), reads=[rt], writes=[rt])
                return
            rpt, r_rp = rp
            a3 = t1[i][:, 0:nh * 32].rearrange("p (n d) -> p n d", n=nh)
            b3 = t2[i][:, 0:nh * 32].rearrange("p (n d) -> p n d", n=nh)
            cos = rpt[:, 96:128].unsqueeze(1).broadcast_to([128, nh, 32])
            sn = rpt[:, 128:144].rearrange("p (h s) -> p h s", h=2).unsqueeze(1).broadcast_to([128, nh, 2, 8])
            S.op("dve", lambda e: e.tensor_tensor(a3, src3, cos, ALU.mult), reads=[rt, r_rp], writes=[rt])
            s5 = src3.rearrange("p n (h t s) -> p n h t s", h=2, t=2)
            b5 = b3.rearrange("p n (h t s) -> p n h t s", h=2, t=2)
            a5 = a3.rearrange("p n (h t s) -> p n h t s", h=2, t=2)
            d5 = dst3.rearrange("p n (h t s) -> p n h t s", h=2, t=2)
            S.op("dve", lambda e: e.tensor_tensor(b5[:, :, :, 0, :], s5[:, :, :, 1, :], sn, ALU.mult), reads=[rt, r_rp], writes=[rt])
            S.op("dve", lambda e: e.tensor_tensor(b5[:, :, :, 1, :], s5[:, :, :, 0, :], sn, ALU.mult), reads=[rt, r_rp], writes=[rt])
            S.op("dve", lambda e: e.tensor_tensor(d5[:, :, :, 0, :], a5[:, :, :, 0, :], b5[:, :, :, 0, :], ALU.subtract), reads=[rt], writes=[rt])
            S.op("dve", lambda e: e.tensor_tensor(d5[:, :, :, 1, :], a5[:, :, :, 1, :], b5[:, :, :, 1, :], ALU.add), reads=[rt], writes=[rt])

        def headnorm(i, src, nh, hd, gain_row, dst):
            rt = res_tiles[i]
            a = t1[i][:, 0:nh * hd]
            S.op("act", lambda e: e.activation(a, src, AF.Square), reads=[rt], writes=[rt])
            s2 = st2[i][:, 0:nh]
            S.op("dve", lambda e: e.tensor_reduce(s2, a.rearrange("p (n d) -> p n d", n=nh), AX.X, ALU.add), reads=[rt], writes=[rt])
            S.op("dve", lambda e: e.tensor_scalar(s2, s2, 1.0 / hd, EPS, ALU.mult, ALU.add), reads=[rt], writes=[rt])
            S.op("act", lambda e: e.activation(s2, s2, AF.Sqrt), reads=[rt], writes=[rt])
            S.op("dve", lambda e: e.reciprocal(s2, s2), reads=[rt], writes=[rt])
            d3 = dst.rearrange("p (n d) -> p n d", n=nh)
            S.op("dve", lambda e: e.tensor_tensor(d3, src.rearrange("p (n d) -> p n d", n=nh), s2.unsqueeze(2).broadcast_to([128, nh, hd]), ALU.mult), reads=[rt], writes=[rt])
            S.op("dve", lambda e: e.tensor_tensor(d3, d3, gain_row.unsqueeze(1).broadcast_to([128, nh, hd]), ALU.mult), reads=[rt, r_sm, r_ckvn], writes=[rt])

        def transpose_to(i, src_bf, ncols, dst, r_dst):
            rt = res_tiles[i]
            tp, r_tp = tp_rot.next()
            S.op("pe", lambda e: e.matmul(tp[0:ncols, 0:128], src_bf, ident_b[:], start=True, stop=True), reads=[rt, r_idb], writes=[r_tp])
            S.op("act", lambda e: e.copy(dst, tp[0:ncols, 0:128]), reads=[r_tp], writes=[r_dst])

        sblocks = []
        for t0 in range(0, n_ctx, 512):
            sblocks.append(("ctx", x_ctx, t0, min(512, n_ctx - t0), t0, (n_own + t0) if not last else None))
        for t0 in range(0, n_own, 512):
            sblocks.append(("own", x_own, t0, 512, n_ctx + t0, t0))
        for t0 in range(0, n_oth, 512):
            sblocks.append(("oth", x_oth, t0, 512, n_ctx + n_own + t0, None))

        for (kind, xsrc, tk0, ntok, kcol, qcol) in sblocks:
            ntile = ntok // 128
            lat = kind != "ctx"
            isq = qcol is not None
            a_i, b_i = (0, 1) if lat else (2, 3)
            rps = []
            for tt in range(ntile):
                xt, r_xt = xt_rot.next()
                S.dma("sp", xt[:], xsrc[tk0 + tt * 128: tk0 + (tt + 1) * 128, :], reads=[r_in], writes=[r_xt])
                ss, r_ss = ss_rot.next()
                S.op("pool", lambda e, ss=ss: e.memset(ss[:], 0.0), writes=[r_ss])
                S.op("act", lambda e, xt=xt, ss=ss: e.activation(sq[:], xt[:], AF.Square, accum_out=ss[:, 0:1]), reads=[r_xt, r_ss], writes=[r_sq, r_ss])
                _rstd_ops(S, ss[:, 0:1], ss[:, 1:2], r_ss, r_ss, 128, 1.0 / D)
                xn, r_xn = xn_rot.next()
                S.op("dve", lambda e, xn=xn, xt=xt, ss=ss: e.tensor_scalar(xn[:], xt[:], ss[:, 1:2], None, ALU.mult), reads=[r_xt, r_ss], writes=[r_xn])
                for half in range(4):
                    tp, r_tp = tp_rot.next()
                    for c in range(4):
                        kc = half * 4 + c
                        S.op("pe", lambda e, tp=tp, xn=xn, c=c, kc=kc: e.matmul(tp[:, c * 128:(c + 1) * 128], xn[:, kc * 128:(kc + 1) * 128], ident_b[:], start=True, stop=True),
                             reads=[r_xn, r_idb], writes=[r_tp])
                    for c in range(4):
                        kc = half * 4 + c
                        S.op("act", lambda e, tp=tp, c=c, kc=kc, tt=tt, a_i=a_i, b_i=b_i: e.activation(
                            uT[:, kc, tt * 128:(tt + 1) * 128], tp[:, c * 128:(c + 1) * 128], AF.Identity,
                            scale=ab[:, a_i, kc:kc + 1], bias=ab[:, b_i, kc:kc + 1]), reads=[r_tp, r_ab], writes=[r_uT])
                if lat:
                    rp = rp_rot.next() if False else None
            if DEBUG and kind == "own" and tk0 == 0:
                udbg, r_udbg = C.dram("udbg", [128, KC, 512], BF16)
                S.dma("sp", udbg, uT[:], reads=[r_uT], writes=[r_udbg])
            blocks = (QBLK if isq else []) + KVBLK
            for (bname, c0, cw) in blocks:
                wt, r_wt = wt_rot.next()
                S.dma("pool", wt[:, :, 0:cw], w_in[:, c0:c0 + cw].rearrange("(c p) n -> p c n", p=128), reads=[r_in], writes=[r_wt])
                for tt in range(ntile):
                    i = tt
                    rt = res_tiles[i]
                    pj, r_pj = pj_rot.next()
                    for kc in range(KC):
                        S.op("pe", lambda e, pj=pj, wt=wt, kc=kc, tt=tt, cw=cw: e.matmul(
                            pj[:, 0:cw], uT[:, kc, tt * 128:(tt + 1) * 128], wt[:, kc, 0:cw], start=(kc == 0), stop=(kc == KC - 1)),
                            reads=[r_uT, r_wt], writes=[r_pj])
                    tok_lat0 = (tk0 if kind == "own" else n_own + tk0) + tt * 128 if lat else 0
                    rp = None
                    if lat and bname in ("AQ", "BQ", "DQ", "AK", "BKV", "DKV", "CKV", "CQ2"):
                        rp = rp_rot.next()
                        S.dma("sp", rp[0][:], rope[tok_lat0:tok_lat0 + 128, :], reads=[r_in], writes=[rp[1]])
                    pr = praw[i]
                    if bname == "CQ2":
                        S.op("act", lambda e, pr=pr, pj=pj: e.copy(pr[:, 512:768], pj[:, 0:256]), reads=[r_pj], writes=[rt])
                    else:
                        S.op("act", lambda e, pr=pr, pj=pj, cw=cw: e.copy(pr[:, 0:cw], pj[:, 0:cw]), reads=[r_pj], writes=[rt])
                    qoff = tt * 128
                    if bname in ("AQ", "DQ", "BQ"):
                        base = {"AQ": 0, "BQ": 8, "DQ": 24}[bname]
                        src = pr[:, 0:512]
                        if bname == "BQ":
                            headnorm(i, src, 8, 64, BQN, t1[i][:, 0:512])
                            S.op("dve", lambda e, pr=pr, i=i: e.tensor_copy(pr[:, 0:512], t1[i][:, 0:512]), reads=[rt], writes=[rt])
                        for hh in range(8):
                            pass
                        if bname == "AQ":
                            half_of = [(hh % 4) % 2 for hh in range(8)]
                        else:
                            half_of = [hh // 4 for hh in range(8)]
                        rope64(i, src, 8, kbf[i][:, 0:512], lat, rp)
                        S.op("pool", lambda e, i=i: e.memset(qpad[i][:], 0.0), reads=[rt], writes=[rt])
                        for hh in range(8):
                            S.op("pool", lambda e, i=i, hh=hh, ho=half_of[hh]: e.tensor_copy(qpad[i][:, hh, ho * 64:(ho + 1) * 64], kbf[i][:, hh * 64:(hh + 1) * 64]), reads=[rt], writes=[rt])
                        for hh in range(8):
                            transpose_to(i, qpad[i][:, hh, :], 128, qstage[:, base + hh, qoff:qoff + 128], r_qst)
                    elif bname == "CQ2":
                        headnorm(i, pr[:, 0:768], 1, 768, CQN, t1[i][:, 0:768])
                        S.op("dve", lambda e, i=i: e.tensor_copy(cqn[i][:], t1[i][:, 0:768]), reads=[rt], writes=[rt])
                        for c in range(6):
                            transpose_to(i, cqn[i][:, c * 128:(c + 1) * 128], 128, cqT[i][:, c, :], rt)
                        for (o0, ow) in ((0, 512), (512, 256)):
                            pq, r_pq = pj_rot.next()
                            for c in range(6):
                                S.op("pe", lambda e, pq=pq, i=i, c=c, o0=o0, ow=ow: e.matmul(pq[:, 0:ow], cqT[i][:, c, :], wq_sb[:, c, o0:o0 + ow], start=(c == 0), stop=(c == 5)),
                                     reads=[rt, r_wq], writes=[r_pq])
                            S.op("act", lambda e, pq=pq, pr=pr, o0=o0, ow=ow: e.copy(pr[:, o0:o0 + ow], pq[:, 0:ow]), reads=[r_pq], writes=[rt])
                        q3 = pr[:, 0:768].rearrange("p (n d) -> p n d", n=8)
                        S.op("dve", lambda e, i=i, q3=q3: e.tensor_copy(c96[i][:, :, 0:64], q3[:, :, 0:64]), reads=[rt], writes=[rt])
                        rope32(i, q3[:, :, 64:96], c96[i][:, :, 64:96], 8, lat, rp)
                        for hh in range(8):
                            transpose_to(i, c96[i][:, hh, :], 96, qstage[0:96, 16 + hh, qoff:qoff + 128], r_qst)
                    elif bname == "CQ1":
                        pass
                    elif bname == "AK":
                        rope64(i, pr[:, 0:512], 8, kbf[i][:, 0:512], lat, rp)
                        for c in range(4):
                            transpose_to(i, kbf[i][:, c * 128:(c + 1) * 128], 128, kstage[:, c, qoff:qoff + 128], r_kst)
                    elif bname == "AV":
                        S.op("dve", lambda e, i=i, pr=pr: e.tensor_copy(vrow[i][:, 0:512], pr[:, 0:512]), reads=[rt], writes=[rt])
                        S.op("dve", lambda e, i=i: e.memset(vrow[i][:, 512:VW].rearrange("p (g d) -> p g d", d=66)[:, :, 0:2], 1.0), reads=[rt], writes=[rt])
                    elif bname in ("BKV", "DKV"):
                        kch = 4 if bname == "BKV" else 13
                        voff = 512 if bname == "BKV" else 512 + 132 + 528
                        if bname == "BKV":
                            headnorm(i, pr[:, 0:128], 2, 64, BKN, t1[i][:, 0:128])
                            S.op("dve", lambda e, pr=pr, i=i: e.tensor_copy(pr[:, 0:128], t1[i][:, 0:128]), reads=[rt], writes=[rt])
                        rope64(i, pr[:, 0:128], 2, kbf[i][:, 0:128], lat, rp)
                        transpose_to(i, kbf[i][:, 0:128], 128, kstage[:, kch, qoff:qoff + 128], r_kst)
                        v3 = vrow[i][:, voff:voff + 132].rearrange("p (g d) -> p g d", g=2)
                        S.op("dve", lambda e, v3=v3, pr=pr: e.tensor_copy(v3[:, :, 2:66], pr[:, 128:256].rearrange("p (g d) -> p g d", g=2)), reads=[rt], writes=[rt])
                    elif bname == "CKV":
                        headnorm(i, pr[:, 0:256], 1, 256, ckvn[:, :], t1[i][:, 0:256])
                        S.op("dve", lambda e, i=i: e.tensor_copy(cqn[i][:, 0:256], t1[i][:, 0:256]), reads=[rt], writes=[rt])
                        S.op("dve", lambda e, i=i, pr=pr: e.tensor_copy(krope[i][:], pr[:, 256:288]), reads=[rt], writes=[rt])
                        for c in range(2):
                            transpose_to(i, cqn[i][:, c * 128:(c + 1) * 128], 128, ckT[i][:, c, :], rt)
                        rope32(i, krope[i][:].unsqueeze(1), t2[i][:, 512:544].unsqueeze(1), 1, lat, rp)
                        S.op("dve", lambda e, i=i: e.tensor_copy(c96[i][:, :, 64:96], t2[i][:, 512:544].unsqueeze(1).broadcast_to([128, 8, 32])), reads=[rt], writes=[rt])
                        voff = 512 + 132
                        for hf in range(2):
                            pq, r_pq = pj_rot.next()
                            for c in range(2):
                                S.op("pe", lambda e, pq=pq, i=i, c=c, hf=hf: e.matmul(pq[:, 0:512], ckT[i][:, c, :], wkv_sb[:, c, hf * 512:(hf + 1) * 512], start=(c == 0), stop=(c == 1)),
                                     reads=[rt, r_wkv], writes=[r_pq])
                            kv3 = pq[:, 0:512].rearrange("p (n d) -> p n d", n=4)
                            S.op("dve", lambda e, i=i, kv3=kv3, hf=hf: e.tensor_copy(c96[i][:, hf * 4:(hf + 1) * 4, 0:64], kv3[:, :, 0:64]), reads=[r_pq, rt], writes=[rt])
                            v3 = vrow[i][:, voff + hf * 264: voff + (hf + 1) * 264].rearrange("p (g d) -> p g d", g=4)
                            S.op("dve", lambda e, v3=v3, kv3=kv3: e.tensor_copy(v3[:, :, 2:66], kv3[:, :, 64:128]), reads=[r_pq, rt], writes=[rt])
                        for hh in range(8):
                            transpose_to(i, c96[i][:, hh, :], 96, kstage[0:96, 5 + hh, qoff:qoff + 128], r_kst)
                    if bname == "DKV":
                        ktile = (kcol // 128) + tt
                        S.dma("sp", vs[:, ktile, :], vrow[i][:], reads=[rt], writes=[r_vs])
            if isq:
                S.dma("sp", qs[:, :, qcol:qcol + ntok].rearrange("c p n -> p c n"), qstage[:, :, 0:ntok], reads=[r_qst], writes=[r_qs])
            S.dma("sp", ks[:, :, kcol:kcol + ntok].rearrange("c p n -> p c n"), kstage[:, :, 0:ntok], reads=[r_kst], writes=[r_ks])
    S.barrier()

    with ExitStack() as ph:
        qt_rot = Rot([C.sb(f"qt{i}", [128, NQ], BF16, ph) for i in range(2)])
        kt_rot = Rot([C.sb(f"kt{i}", [128, NK], BF16, ph) for i in range(2)])
        vt_rot = Rot([C.sb(f"vt{i}", [128, NKT, 128], BF16, ph) for i in range(2)])
        p_rot = Rot([C.sb(f"pb{i}", [128, 512], BF16, ph) for i in range(4)])
        sps_rot = Rot([C.ps(f"sps{i}", [128, 512], F32, ph) for i in range(4)])
        ops_rot = Rot([C.ps(f"ops{i}", [128, 512], F32, ph) for i in range(2)])
        sum_rot = Rot([C.ps(f"sums{i}", [128, 512], F32, ph) for i in range(2)])
        mk, r_mk = C.sb("mk", [128, 8, 512], BF16, ph)
        S.dma("pool", mk[:], masks.rearrange("m p n -> p m n"), reads=[r_in], writes=[r_mk])
        rs_rot = Rot([C.sb(f"rs{i}", [128, 512], F32, ph) for i in range(2)])
        bc_rot = Rot([C.sb(f"bc{i}", [128, 512], F32, ph) for i in range(2)])
        ob_rot = Rot([C.sb(f"ob{i}", [128, 512], BF16, ph) for i in range(3)])
        a1_rot = Rot([C.sb(f"a1_{i}", [128, 512], F32, ph) for i in range(NQB + (0 if last else 1))])
        a2_rot = Rot([C.sb(f"a2_{i}", [128, 512], F32, ph) for i in range(2)])

        loaded = {"q": None, "k": None, "v": None}

        def load_q(ch):
            qt, r_qt = qt_rot.next()
            S.dma("sp", qt[:], qs[ch], reads=[r_qs], writes=[r_qt])
            return qt, r_qt

        def load_k(ch):
            if loaded["k"] is not None and loaded["k"][0] == ch:
                return loaded["k"][1]
            kt, r_kt = kt_rot.next()
            S.dma("sp", kt[:], ks[ch], reads=[r_ks], writes=[r_kt])
            loaded["k"] = (ch, (kt, r_kt))
            return kt, r_kt

        def load_v(voff, vw):
            key = (voff, vw)
            if loaded["v"] is not None and loaded["v"][0] == key:
                return loaded["v"][1]
            vt, r_vt = vt_rot.next()
            S.dma("sp", vt[:, :, 0:vw], vs[:, :, voff:voff + vw], reads=[r_vs], writes=[r_vt])
            loaded["v"] = (key, (vt, r_vt))
            return vt, r_vt

        def attend(qt, r_qt, kt, r_kt, vt, r_vt, vw, dk, q0, nq, ktiles, scale, ones_sum):
            ops, r_ops = ops_rot.next()
            sums, r_sums = sum_rot.next() if ones_sum else (None, None)
            n = len(ktiles)
            pend = []

            def qk(j):
                kti, mi = ktiles[j]
                sp_, r_sp = sps_rot.next()
                S.op("pe", lambda e: e.matmul(sp_[:, 0:nq], kt[0:dk, kti * 128:(kti + 1) * 128], qt[0:dk, q0:q0 + nq], start=True, stop=True),
                     reads=[r_kt, r_qt], writes=[r_sp])
                pb, r_pb = p_rot.next()
                S.op("act", lambda e: e.activation(pb[:, 0:nq], sp_[:, 0:nq], AF.Exp, scale=scale), reads=[r_sp], writes=[r_pb])
                if mi is not None:
                    S.op("dve", lambda e: e.tensor_tensor(pb[:, 0:nq], pb[:, 0:nq], mk[:, mi, 0:nq], ALU.mult), reads=[r_pb, r_mk], writes=[r_pb])
                pend.append((j, kti, pb, r_pb))

            def pv():
                j, kti, pb, r_pb = pend.pop(0)
                S.op("pe", lambda e: e.matmul(ops[0:vw, 0:nq], vt[:, kti, 0:vw], pb[:, 0:nq], start=(j == 0), stop=(j == n - 1)),
                     reads=[r_vt, r_pb], writes=[r_ops])
                if ones_sum:
                    S.op("pe", lambda e: e.matmul(sums[:, 0:nq], ones_b[:], pb[:, 0:nq], start=(j == 0), stop=(j == n - 1)),
                         reads=[r_ones, r_pb], writes=[r_sums])

            for j in range(n):
                qk(j)
                if j >= 2:
                    pv()
            while pend:
                pv()
            return ops, r_ops, sums, r_sums

        def finish65(ops, r_ops, nq, q0, row0, sink_h):
            rs, r_rs = rs_rot.next()
            if sink_h is not None:
                S.op("dve", lambda e: e.tensor_scalar(rs[0:1, 0:nq], ops[0:1, 0:nq], sinkexp[0:1, sink_h:sink_h + 1], None, ALU.add), reads=[r_ops, r_sink], writes=[r_rs])
                S.op("dve", lambda e: e.reciprocal(rs[0:1, 0:nq], rs[0:1, 0:nq]), reads=[r_rs], writes=[r_rs])
            else:
                S.op("dve", lambda e: e.reciprocal(rs[0:1, 0:nq], ops[0:1, 0:nq]), reads=[r_ops], writes=[r_rs])
            bp, r_bp = sps_rot.next()
            S.op("pe", lambda e: e.matmul(bp[0:66, 0:nq], ones_f[0:1, 0:66], rs[0:1, 0:nq], start=True, stop=True), reads=[r_onesf, r_rs], writes=[r_bp])
            bc, r_bc = bc_rot.next()
            S.op("act", lambda e: e.copy(bc[0:66, 0:nq], bp[0:66, 0:nq]), reads=[r_bp], writes=[r_bc])
            ob, r_ob = ob_rot.next()
            S.op("dve", lambda e: e.tensor_tensor(ob[0:66, 0:nq], ops[0:66, 0:nq], bc[0:66, 0:nq], ALU.mult), reads=[r_ops, r_bc], writes=[r_ob])
            S.dma("pool", mixT[row0:row0 + 64, q0:q0 + nq], ob[2:66, 0:nq], reads=[r_ob], writes=[r_mix])

        dense_lat = [(kt_, None) for kt_ in range(NKT)]
        dense_ctx = [(kt_, None) for kt_ in range(CT)]
        qblocks = [(qb * 512, 512, False, qb) for qb in range(NQB)]
        if not last:
            qblocks.append((n_own, n_ctx, True, None))

        for h in range(4):
            vt, r_vt = load_v(h * 128, 128)
            res_a = {}
            for m in range(2):
                hm = m * 4 + h
                qt, r_qt = load_q(hm)
                kt, r_kt = load_k(m * 2 + h // 2)
                for (q0, nq, isc, qb) in qblocks:
                    ops, r_ops, sums, r_sums = attend(qt, r_qt, kt, r_kt, vt, r_vt, 128, 128, q0, nq,
                                                      dense_ctx if isc else dense_lat, 0.125, True)
                    rs, r_rs = rs_rot.next()
                    S.op("dve", lambda e, rs=rs, sums=sums, nq=nq: e.reciprocal(rs[:, 0:nq], sums[:, 0:nq]), reads=[r_sums], writes=[r_rs])
                    if m == 0:
                        a1, r_a1 = a1_rot.next()
                        S.op("dve", lambda e, a1=a1, ops=ops, rs=rs, nq=nq: e.tensor_tensor(a1[:, 0:nq], ops[:, 0:nq], rs[:, 0:nq], ALU.mult), reads=[r_ops, r_rs], writes=[r_a1])
                        res_a[q0] = (a1, r_a1)
                    else:
                        a1, r_a1 = res_a[q0]
                        a2, r_a2 = a2_rot.next()
                        S.op("dve", lambda e, a2=a2, ops=ops, rs=rs, nq=nq: e.tensor_tensor(a2[:, 0:nq], ops[:, 0:nq], rs[:, 0:nq], ALU.mult), reads=[r_ops, r_rs], writes=[r_a2])
                        S.op("dve", lambda e, a1=a1, a2=a2, nq=nq: e.scalar_tensor_tensor(a1[:, 0:nq], a2[:, 0:nq], NEG_LAM, a1[:, 0:nq], ALU.mult, ALU.add), reads=[r_a1, r_a2, r_lam], writes=[r_a1])
                        S.op("act", lambda e, a2=a2, a1=a1, nq=nq: e.activation(a2[:, 0:nq], a1[:, 0:nq], AF.Square), reads=[r_a1], writes=[r_a2])
                        bp, r_bp = sps_rot.next()
                        S.op("pe", lambda e, bp=bp, a2=a2, nq=nq: e.matmul(bp[:, 0:nq], ones_f[:], a2[:, 0:nq], start=True, stop=True), reads=[r_onesf, r_a2], writes=[r_bp])
                        S.op("dve", lambda e, a2=a2, bp=bp, nq=nq: e.tensor_scalar(a2[:, 0:nq], bp[:, 0:nq], 1.0 / 128, EPS, ALU.mult, ALU.add), reads=[r_bp], writes=[r_a2])
                        S.op("act", lambda e, a2=a2, nq=nq: e.activation(a2[:, 0:nq], a2[:, 0:nq], AF.Sqrt), reads=[r_a2], writes=[r_a2])
                        S.op("dve", lambda e, a2=a2, nq=nq: e.reciprocal(a2[:, 0:nq], a2[:, 0:nq]), reads=[r_a2], writes=[r_a2])
                        ob, r_ob = ob_rot.next()
                        S.op("dve", lambda e, ob=ob, a1=a1, a2=a2, nq=nq: e.scalar_tensor_tensor(ob[:, 0:nq], a1[:, 0:nq], subg[:, 0:1], a2[:, 0:nq], ALU.mult, ALU.mult), reads=[r_a1, r_a2, r_subg], writes=[r_ob])
                        S.dma("pool", mixT[h * 128:(h + 1) * 128, q0:q0 + nq], ob[:, 0:nq], reads=[r_ob], writes=[r_mix])
        for h in range(8):
            g = h // 4
            vt, r_vt = load_v(512 + g * 66, 66)
            qt, r_qt = load_q(8 + h)
            kt, r_kt = load_k(4)
            for (q0, nq, isc, qb) in qblocks:
                ops, r_ops, _, _ = attend(qt, r_qt, kt, r_kt, vt, r_vt, 66, 128, q0, nq, dense_ctx if isc else dense_lat, 0.125, False)
                finish65(ops, r_ops, nq, q0, 512 + h * 64, None)
        for h in range(8):
            vt, r_vt = load_v(512 + 132 + h * 66, 66)
            qt, r_qt = load_q(16 + h)
            kt, r_kt = load_k(5 + h)
            for (q0, nq, isc, qb) in qblocks:
                ops, r_ops, _, _ = attend(qt, r_qt, kt, r_kt, vt, r_vt, 66, 96, q0, nq, dense_ctx if isc else dense_lat, 96 ** -0.5, False)
                finish65(ops, r_ops, nq, q0, 1024 + h * 64, None)
        for h in range(8):
            g = h // 4
            vt, r_vt = load_v(512 + 132 + 528 + g * 66, 66)
            qt, r_qt = load_q(24 + h)
            kt, r_kt = load_k(13)
            for (q0, nq, isc, qb) in qblocks:
                if isc:
                    kts = dense_ctx
                else:
                    kts = [(kt_, None) for kt_ in range(CT)]
                    for r in range(-1, 5):
                        ot_ = qb * 4 + r
                        if 0 <= ot_ < OT:
                            kts.append((CT + ot_, r + 1))
                        elif ot_ == -1:
                            kts.append((CT + OT, 6))
                        elif ot_ == OT:
                            kts.append((CT + OT, 7))
                ops, r_ops, _, _ = attend(qt, r_qt, kt, r_kt, vt, r_vt, 66, 128, q0, nq, kts, 0.125, False)
                finish65(ops, r_ops, nq, q0, 1536 + h * 64, h)
    S.barrier()

    if moe:
        hmid_d, r_hmid_d, u2t_d, r_u2t_d = hmid_out, r_hmid, u2t_out, r_u2t
    else:
        hmid_d, r_hmid_d = C.dram("hmid_s", [NQ, D], F32)
        u2t_d, r_u2t_d = C.dram("u2t_s", [D, NQ], BF16)
    with ExitStack() as ph:
        T = 512
        wo_rot = Rot([C.sb(f"wo{i}", [128, KC, 512], BF16, ph) for i in range(2)])
        vb, r_vb = C.sb("vb", [128, 2, D], F32, ph)
        S.dma("sp", vb[:], vbc[:, 0:2, :], reads=[r_in], writes=[r_vb])
        mx_rot = Rot([C.sb(f"mx{i}", [128, KC, T], BF16, ph) for i in range(1)])
        xt_rot = Rot([C.sb(f"x3_{i}", [128, D], F32, ph) for i in range(2)])
        hm_rot = Rot([C.sb(f"hm{i}", [128, D], F32, ph) for i in range(4)])
        sq, r_sq = C.sb("sq3", [128, D], F32, ph)
        xn_rot = Rot([C.sb(f"xn3_{i}", [128, D], BF16, ph) for i in range(1)])
        ss_rot = Rot([C.sb(f"ss3_{i}", [128, 2], F32, ph) for i in range(2)])
        u2_rot = Rot([C.sb(f"u2T{i}", [128, KC, T], BF16, ph) for i in range(1)])
        wo_ps = Rot([C.ps(f"wops{i}", [128, 512], F32, ph) for i in range(4)])
        tp_rot = Rot([C.ps(f"tp3_{i}", [128, 512], F32, ph) for i in range(2)])
        if moe:
            vb2, r_vb2 = C.sb("vb2", [128, 3, D], F32, ph)
            S.dma("sp", vb2[:], vbc[:, 3:6, :], reads=[r_in], writes=[r_vb2])
            S.op("dve", lambda e: e.tensor_scalar(vb2[:, 1, :], vb2[:, 1, :], 1.0, None, ALU.add), reads=[r_vb2], writes=[r_vb2])
            S.op("dve", lambda e: e.tensor_tensor(vb2[:, 1, :], vb2[:, 1, :], vb2[:, 0, :], ALU.mult), reads=[r_vb2], writes=[r_vb2])
            wr_rot = Rot([C.sb(f"wrs{i}", [128, D], F32, ph) for i in range(2)])
            u2f, r_u2f = C.sb("u2f", [128, D], F32, ph)
            lg_rot = Rot([C.sb(f"lg{i}", [128, 64], F32, ph) for i in range(2)])
            brow = sm[:, 1296:1304]

        groups = [("own", x_own, t0, 512, t0) for t0 in range(0, n_own, 512)]
        if not last:
            groups += [("ctx", x_ctx, t0, min(512, n_ctx - t0), n_own + t0) for t0 in range(0, n_ctx, 512)]
        for (kind, xsrc, tk0, ntok, qcol) in groups:
            ntile = ntok // 128
            isc = kind == "ctx"
            gate_b = vb[:, 1 if isc else 0, :]
            a_i, b_i = (6, 7) if isc else (4, 5)
            mx, r_mx = mx_rot.next()
            S.dma("sp", mx[:, :, 0:ntok], mixT[:, qcol:qcol + ntok].rearrange("(c p) n -> p c n", p=128), reads=[r_mix], writes=[r_mx])
            u2T, r_u2T = u2_rot.next()
            tiles = []
            for tt in range(ntile):
                hm, r_hm = hm_rot.next()
                tiles.append((None, None, hm, r_hm))
            for cb in range(4):
                wo, r_wo = wo_rot.next()
                S.dma("pool", wo[:], w_out[:, cb * 512:(cb + 1) * 512].rearrange("(c p) n -> p c n", p=128), reads=[r_in], writes=[r_wo])
                for tt in range(ntile):
                    xt, r_xt, hm, r_hm = tiles[tt]
                    wp, r_wp = wo_ps.next()
                    for kc in range(KC):
                        S.op("pe", lambda e, wp=wp, mx=mx, kc=kc, tt=tt, wo=wo: e.matmul(
                            wp[:, :], mx[:, kc, tt * 128:(tt + 1) * 128], wo[:, kc, :], start=(kc == 0), stop=(kc == KC - 1)),
                            reads=[r_mx, r_wo], writes=[r_wp])
                    S.op("dve", lambda e, hm=hm, wp=wp, cb=cb, gate_b=gate_b: e.tensor_tensor(hm[:, cb * 512:(cb + 1) * 512], wp[:, :], gate_b[:, cb * 512:(cb + 1) * 512], ALU.mult),
                         reads=[r_wp, r_vb], writes=[r_hm])
            for tt in range(ntile):
                _, _, hm, r_hm = tiles[tt]
                xt, r_xt = xt_rot.next()
                S.dma("sp", xt[:], xsrc[tk0 + tt * 128: tk0 + (tt + 1) * 128, :], reads=[r_in], writes=[r_xt])
                row0 = qcol + tt * 128
                S.op("pool", lambda e, hm=hm, xt=xt: e.tensor_tensor(hm[:], hm[:], xt[:], ALU.add), reads=[r_hm, r_xt], writes=[r_hm])
                S.dma("sp", hmid_d[row0:row0 + 128, :], hm[:], reads=[r_hm], writes=[r_hmid_d])
                ss, r_ss = ss_rot.next()
                S.op("pool", lambda e, ss=ss: e.memset(ss[:], 0.0), writes=[r_ss])
                S.op("act", lambda e, hm=hm, ss=ss: e.activation(sq[:], hm[:], AF.Square, accum_out=ss[:, 0:1]), reads=[r_hm, r_ss], writes=[r_sq, r_ss])
                _rstd_ops(S, ss[:, 0:1], ss[:, 1:2], r_ss, r_ss, 128, 1.0 / D)
                xn, r_xn = xn_rot.next()
                S.op("dve", lambda e, xn=xn, hm=hm, ss=ss: e.tensor_scalar(xn[:], hm[:], ss[:, 1:2], None, ALU.mult), reads=[r_hm, r_ss], writes=[r_xn])
                for half in range(4):
                    tp, r_tp = tp_rot.next()
                    for c in range(4):
                        kc = half * 4 + c
                        S.op("pe", lambda e, tp=tp, xn=xn, c=c, kc=kc: e.matmul(tp[:, c * 128:(c + 1) * 128], xn[:, kc * 128:(kc + 1) * 128], ident_b[:], start=True, stop=True),
                             reads=[r_xn, r_idb], writes=[r_tp])
                    for c in range(4):
                        kc = half * 4 + c
                        S.op("act", lambda e, tp=tp, c=c, kc=kc, tt=tt, u2T=u2T, a_i=a_i, b_i=b_i: e.activation(
                            u2T[:, kc, tt * 128:(tt + 1) * 128], tp[:, c * 128:(c + 1) * 128], AF.Identity,
                            scale=ab[:, a_i, kc:kc + 1], bias=ab[:, b_i, kc:kc + 1]), reads=[r_tp, r_ab], writes=[r_u2T])
                if moe:
                    S.op("dve", lambda e, hm=hm, ss=ss: e.scalar_tensor_tensor(u2f[:], hm[:], ss[:, 1:2], vb2[:, 1, :], ALU.mult, ALU.mult), reads=[r_hm, r_ss, r_vb2], writes=[r_u2f])
                    S.op("pool", lambda e: e.tensor_tensor(u2f[:], u2f[:], vb2[:, 2, :], ALU.add), reads=[r_u2f, r_vb2], writes=[r_u2f])
                    lg, r_lg = lg_rot.next()
                    for ex in range(NEXP):
                        wrs, r_wrs = wr_rot.next()
                        S.dma("sp", wrs[:], wr[:, ex, :], reads=[r_in], writes=[r_wrs])
                        S.op("dve", lambda e, wrs=wrs: e.tensor_tensor(sq[:], u2f[:], wrs[:], ALU.mult), reads=[r_u2f, r_wrs], writes=[r_sq])
                        S.op("dve", lambda e, ex=ex, lg=lg: e.tensor_reduce(lg[:, ex:ex + 1], sq[:], AX.X, ALU.add), reads=[r_sq], writes=[r_lg])
                    S.op("dve", lambda e, lg=lg: e.tensor_tensor(lg[:, 0:8], lg[:, 0:8], brow, ALU.add), reads=[r_lg, r_sm], writes=[r_lg])
                    S.op("dve", lambda e, lg=lg: e.tensor_reduce(lg[:, 8:9], lg[:, 0:8], AX.X, ALU.max), reads=[r_lg], writes=[r_lg])
                    S.op("dve", lambda e, lg=lg: e.tensor_scalar(lg[:, 16:24], lg[:, 0:8], lg[:, 8:9], None, ALU.is_equal), reads=[r_lg], writes=[r_lg])
                    S.op("dve", lambda e, lg=lg: e.scalar_tensor_tensor(lg[:, 24:32], lg[:, 16:24], -1e30, lg[:, 0:8], ALU.mult, ALU.add), reads=[r_lg], writes=[r_lg])
                    S.op("dve", lambda e, lg=lg: e.tensor_reduce(lg[:, 9:10], lg[:, 24:32], AX.X, ALU.max), reads=[r_lg], writes=[r_lg])
                    S.op("dve", lambda e, lg=lg: e.tensor_scalar(lg[:, 32:40], lg[:, 24:32], lg[:, 9:10], None, ALU.is_equal), reads=[r_lg], writes=[r_lg])
                    S.op("dve", lambda e, lg=lg: e.tensor_tensor(lg[:, 10:11], lg[:, 8:9], lg[:, 9:10], ALU.subtract), reads=[r_lg], writes=[r_lg])
                    S.op("act", lambda e, lg=lg: e.activation(lg[:, 11:12], lg[:, 10:11], AF.Sigmoid), reads=[r_lg], writes=[r_lg])
                    S.op("dve", lambda e, lg=lg: e.tensor_scalar(lg[:, 12:13], lg[:, 11:12], -1.0, 1.0, ALU.mult, ALU.add), reads=[r_lg], writes=[r_lg])
                    S.op("dve", lambda e, lg=lg: e.tensor_scalar(lg[:, 40:48], lg[:, 16:24], lg[:, 11:12], None, ALU.mult), reads=[r_lg], writes=[r_lg])
                    S.op("dve", lambda e, lg=lg: e.scalar_tensor_tensor(lg[:, 48:56], lg[:, 32:40], lg[:, 12:13], lg[:, 40:48], ALU.mult, ALU.add), reads=[r_lg], writes=[r_lg])
                    S.dma("sp", gates_out[tk0 + tt * 128: tk0 + (tt + 1) * 128, :], lg[:, 48:56], reads=[r_lg], writes=[r_gates])
            S.dma("sp", u2t_d[:, qcol:qcol + ntok].rearrange("(c p) n -> p c n", p=128), u2T[:, :, 0:ntok], reads=[r_u2T], writes=[r_u2t_d])
    S.barrier()

    if not moe:
        with ExitStack() as ph:
            T = 1024
            ident_f2, r_idf2 = ident_f, r_idf
            ffn = FFN(C, T, wg, wu, wd, r_in, ph)
            u2_rot = Rot([C.sb(f"fu2T{i}", [128, KC, T], BF16, ph) for i in range(1)])
            ysb_rot = Rot([C.sb(f"ysb{i}", [128, 512], F32, ph) for i in range(2)])
            ytp_rot = Rot([C.ps(f"ytp{i}", [128, 512], F32, ph) for i in range(2)])
            po_rot = Rot([C.sb(f"po{i}", [128, 512], F32, ph) for i in range(2)])
            hb_rot = Rot([C.sb(f"hb{i}", [128, 512], F32, ph) for i in range(2)])
            fgroups = [(t0, min(T, n_own - t0), False) for t0 in range(0, n_own, T)]
            if not last:
                fgroups += [(n_own + t0, min(T, n_ctx - t0), True) for t0 in range(0, n_ctx, T)]
            for (q0, ntok, isc) in fgroups:
                u2T, r_u2T = u2_rot.next()
                S.dma("sp", u2T[:, :, 0:ntok], u2t_d[:, q0:q0 + ntok].rearrange("(c p) n -> p c n", p=128), reads=[r_u2t_d], writes=[r_u2T])

                def sink(cc, tb, yp, r_yp, tw, q0=q0, isc=isc):
                    ysb, r_ysb = ysb_rot.next()
                    gi = 11 if isc else 10
                    S.op("act", lambda e: e.activation(ysb[:, 0:tw], yp[:, 0:tw], AF.Copy, scale=vp[:, gi, cc:cc + 1]), reads=[r_yp, r_vp], writes=[r_ysb])
                    yt, r_yt = ytp_rot.next()
                    nt4 = tw // 128
                    for tt in range(nt4):
                        S.op("pe", lambda e, tt=tt: e.matmul(yt[:, tt * 128:(tt + 1) * 128], ysb[:, tt * 128:(tt + 1) * 128], ident_f[:], start=True, stop=True), reads=[r_ysb, r_idf], writes=[r_yt])
                    tok0 = q0 + tb * 512
                    hb, r_hb = hb_rot.next()
                    v3 = lambda ap: ap.rearrange("p (t f) -> p t f", t=nt4)
                    S.dma("sp", v3(hb[:, 0:tw]), hmid_d[tok0:tok0 + tw, cc * 128:(cc + 1) * 128].rearrange("(t p) f -> p t f", p=128), reads=[r_hmid_d], writes=[r_hb])
                    po, r_po = po_rot.next()
                    S.op("dve", lambda e: e.tensor_tensor(po[:, 0:tw], yt[:, 0:tw], hb[:, 0:tw], ALU.add), reads=[r_yt, r_hb], writes=[r_po])
                    if isc:
                        dst, r_dst, r0 = hctx_out, r_hctx, tok0 - n_own
                    else:
                        dst, r_dst, r0 = h_out, r_hout, tok0
                    S.dma("sp", dst[r0:r0 + tw, cc * 128:(cc + 1) * 128].rearrange("(t p) f -> p t f", p=128), v3(po[:, 0:tw]), reads=[r_po], writes=[r_dst])

                ffn.run(u2T, r_u2T, ntok, sink)
    S.emit()
    C.st.close()
    return C.nc


def build_moe(nt, T):
    C = Ctx()
    S = C.S
    u2t, r_in = C.ext_in("u2t", [D, nt], BF16)
    gate, _ = C.ext_in("gate", [128, nt // 128])
    wg, _ = C.ext_in("wg", [D, DFF])
    wu, _ = C.ext_in("wu", [D, DFF])
    wd, _ = C.ext_in("wd", [DFF, D])
    ident_in, _ = C.ext_in("ident", [128, 128])
    part, r_part = C.ext_out("part", [nt, D])
    ident_f, r_idf = C.sb("ident_f", [128, 128], F32)
    S.dma("sp", ident_f[:], ident_in, reads=[r_in], writes=[r_idf])
    gt, r_gt = C.sb("gt", [128, nt // 128], F32)
    S.dma("sp", gt[:], gate, reads=[r_in], writes=[r_gt])
    ffn = FFN(C, T, wg, wu, wd, r_in)
    u2_rot = Rot([C.sb(f"u2T{i}", [128, KC, T], BF16) for i in range(1)])
    ysb_rot = Rot([C.sb(f"ysb{i}", [128, 512], F32) for i in range(2)])
    ytp_rot = Rot([C.ps(f"ytp{i}", [128, 512], F32) for i in range(2)])
    po_rot = Rot([C.sb(f"po{i}", [128, 512], F32) for i in range(2)])
    for t0 in range(0, nt, T):
        ntok = min(T, nt - t0)
        u2T, r_u2T = u2_rot.next()
        S.dma("sp", u2T[:, :, 0:ntok], u2t[:, t0:t0 + ntok].rearrange("(c p) n -> p c n", p=128), reads=[r_in], writes=[r_u2T])

        def sink(cc, tb, yp, r_yp, tw, t0=t0):
            ysb, r_ysb = ysb_rot.next()
            S.op("act", lambda e: e.copy(ysb[:, 0:tw], yp[:, 0:tw]), reads=[r_yp], writes=[r_ysb])
            yt, r_yt = ytp_rot.next()
            nt4 = tw // 128
            for tt in range(nt4):
                S.op("pe", lambda e, tt=tt: e.matmul(yt[:, tt * 128:(tt + 1) * 128], ysb[:, tt * 128:(tt + 1) * 128], ident_f[:], start=True, stop=True), reads=[r_ysb, r_idf], writes=[r_yt])
            po, r_po = po_rot.next()
            tile0 = (t0 + tb * 512) // 128
            S.op("dve", lambda e: e.tensor_tensor(po[:, 0:tw].rearrange("p (t f) -> p t f", t=nt4), yt[:, 0:tw].rearrange("p (t f) -> p t f", t=nt4),
                                                  gt[:, tile0:tile0 + nt4].unsqueeze(2).broadcast_to([128, nt4, 128]), ALU.mult), reads=[r_yt, r_gt], writes=[r_po])
            tok0 = t0 + tb * 512
            S.dma("sp", part[tok0:tok0 + tw, cc * 128:(cc + 1) * 128].rearrange("(t p) f -> p t f", p=128), po[:, 0:tw].rearrange("p (t f) -> p t f", t=nt4),
                  reads=[r_po], writes=[r_part])

        ffn.run(u2T, r_u2T, ntok, sink)
    S.emit()
    C.st.close()
    return C.nc


def build_comb(n_own, nparts):
    C = Ctx()
    S = C.S
    hmid, r_in = C.ext_in("hmid", [n_own, D])
    vb_in, _ = C.ext_in("vb", [128, 2, D])
    if nparts:
        parts, _ = C.ext_in("parts", [nparts, n_own, D])
    out, r_out = C.ext_out("out", [n_own, D])
    vb, r_vb = C.sb("vb_sb", [128, 2, D], F32)
    S.dma("sp", vb[:], vb_in, reads=[r_in], writes=[r_vb])
    h_rot = Rot([C.sb(f"h{i}", [128, D], F32) for i in range(2)])
    p_rot = Rot([C.sb(f"p{i}", [128, D], F32) for i in range(4)])
    acc_rot = Rot([C.sb(f"acc{i}", [128, D], F32) for i in range(2)])
    sq, r_sq = C.sb("sq", [128, D], F32)
    ss_rot = Rot([C.sb(f"ss{i}", [128, 2], F32) for i in range(2)])
    o_rot = Rot([C.sb(f"o{i}", [128, D], F32) for i in range(2)])
    for tt in range(n_own // 128):
        rows = slice(tt * 128, (tt + 1) * 128)
        h, r_h = h_rot.next()
        S.dma("sp", h[:], hmid[rows, :], reads=[r_in], writes=[r_h])
        if nparts:
            acc, r_acc = acc_rot.next()
            for ex in range(nparts):
                p_, r_p = p_rot.next()
                S.dma("pool" if ex % 2 else "sp", p_[:], parts[ex, rows, :], reads=[r_in], writes=[r_p])
                if ex == 0:
                    S.op("pool", lambda e, acc=acc, p_=p_: e.tensor_copy(acc[:], p_[:]), reads=[r_p], writes=[r_acc])
                else:
                    eng = "pool" if ex % 2 else "dve"
                    S.op(eng, lambda e, acc=acc, p_=p_: e.tensor_tensor(acc[:], acc[:], p_[:], ALU.add), reads=[r_p, r_acc], writes=[r_acc])
            S.op("dve", lambda e, acc=acc: e.tensor_tensor(acc[:], acc[:], vb[:, 0, :], ALU.mult), reads=[r_acc, r_vb], writes=[r_acc])
            S.op("dve", lambda e, acc=acc, h=h: e.tensor_tensor(h[:], h[:], acc[:], ALU.add), reads=[r_acc, r_h], writes=[r_h])
        ss, r_ss = ss_rot.next()
        S.op("pool", lambda e, ss=ss: e.memset(ss[:], 0.0), writes=[r_ss])
        S.op("act", lambda e, h=h, ss=ss: e.activation(sq[:], h[:], AF.Square, accum_out=ss[:, 0:1]), reads=[r_h, r_ss], writes=[r_sq, r_ss])
        _rstd_ops(S, ss[:, 0:1], ss[:, 1:2], r_ss, r_ss, 128, 1.0 / D)
        o, r_o = o_rot.next()
        S.op("dve", lambda e, o=o, h=h, ss=ss: e.scalar_tensor_tensor(o[:], h[:], ss[:, 1:2], vb[:, 1, :], ALU.mult, ALU.mult), reads=[r_h, r_ss, r_vb], writes=[r_o])
        S.dma("sp", out[rows, :], o[:], reads=[r_o], writes=[r_out])
    S.emit()
    C.st.close()
    return C.nc


def _pc(v):
    return np.ascontiguousarray(np.asarray(v, np.float32).reshape(KC, 128).T)


def _bc(v, n=128):
    v = np.asarray(v, np.float32).reshape(1, -1)
    return np.ascontiguousarray(np.broadcast_to(v, (n, v.shape[1])))


def _rope_rows(pos):
    rows = (pos // GRID_W).astype(np.float32)
    cols = (pos % GRID_W).astype(np.float32)

    def tab(axis_dim):
        inv = (10000.0 ** (-np.arange(0, axis_dim, 2, dtype=np.float32) / axis_dim)).astype(np.float32)
        ar = rows[:, None] * inv[None, :]
        ac = cols[:, None] * inv[None, :]
        return np.cos(ar), np.sin(ar), np.cos(ac), np.sin(ac)

    cr, sr, cc, sc = tab(32)
    crm, srm, ccm, scm = tab(16)
    return np.ascontiguousarray(np.concatenate([cr, cr, cc, cc, sr, sc, crm, crm, ccm, ccm, srm, scm], axis=1).astype(np.float32))


def _masks(half, n_halves):
    m = np.zeros((8, 128, 512), np.float32)
    k = np.arange(128)[:, None]
    q = np.arange(512)[None, :]
    for r in range(-1, 5):
        m[r + 1] = (np.abs(q - (r * 128 + k)) <= 128)
    if n_halves == 2:
        if half == 1:
            m[6] = m[0]
        if half == 0:
            m[7] = m[5]
    return m


_CACHE = {}


def _prog(key, fn):
    if key not in _CACHE:
        _CACHE[key] = fn()
    return _CACHE[key]


def _run(nc, in_maps):
    res = run_bass_kernel_spmd(nc, in_maps, core_ids=list(range(len(in_maps))))
    return res.results


def kernel(x, c, ctx, c_ctx, w_mod, b_mod, g_mix, g_ffn, g_final, w_in, w_out,
           a_lam_q1, a_lam_k1, a_lam_q2, a_lam_k2, a_subln, b_q_norm, b_k_norm,
           c_q_norm, c_kv_norm, c_w_q_up, c_w_kv_up, d_sink,
           ffn_w_gate, ffn_w_up, ffn_w_down,
           moe_w_router, moe_b_router, moe_w_gate, moe_w_up, moe_w_down):
    f = lambda a: np.asarray(a, np.float32)
    x, c, ctx, c_ctx = f(x), f(c), f(ctx), f(c_ctx)
    B, SEQ, _ = x.shape
    n_ctx = ctx.shape[1]
    depth = w_in.shape[0]
    NH = 2 if B * 2 <= 8 else 1
    n_own = SEQ // NH
    n_oth = SEQ - n_own
    ident = np.eye(128, dtype=np.float32)

    nv = B + 1
    cvec = np.concatenate([c, c_ctx[None, :]], axis=0)
    ct = np.zeros((128, KC, 5), np.float32)
    ct[:, :, :nv] = cvec.reshape(nv, KC, 128).transpose(2, 1, 0)
    ncols = 6 * D // 8
    w_mod = f(w_mod)
    b_mod = f(b_mod)
    if depth == 1:
        w_mod = np.concatenate([w_mod, w_mod], 0)
        b_mod = np.concatenate([b_mod, b_mod], 0)
    nc_mod = _prog(("mod", ncols), lambda: build_mod(ncols))
    maps = []
    for j in range(8):
        cs = slice(j * ncols, (j + 1) * ncols)
        maps.append({"w": np.ascontiguousarray(w_mod[:, :, cs]),
                     "b": np.ascontiguousarray(np.broadcast_to(b_mod[:, None, cs], (2, 5, ncols))),
                     "ct": ct})
    r = _run(nc_mod, maps)
    mod = np.stack([np.concatenate([r[j][f"out{l}"] for j in range(8)], axis=1) for l in range(2)], axis=0)

    h_lat = x
    h_ctx = ctx
    out = None
    for l in range(depth):
        last = l == depth - 1
        moe = l % 2 == 1
        lam_init = 0.8 - 0.6 * math.exp(-0.3 * l)
        nc_l = _prog(("layer", last, moe, n_own, n_oth, n_ctx, l), lambda: build_layer(last, moe, n_own, n_oth, n_ctx, lam_init))
        small = np.zeros((1304,), np.float32)
        small[0:64] = f(a_lam_q1)[l]
        small[64:128] = f(a_lam_k1)[l]
        small[128:192] = f(a_lam_q2)[l]
        small[192:256] = f(a_lam_k2)[l]
        small[384:448] = f(b_q_norm)[l]
        small[448:512] = f(b_k_norm)[l]
        small[512:1280] = f(c_q_norm)[l]
        small[1280:1288] = f(d_sink)[l]
        if moe:
            small[1296:1304] = f(moe_b_router)[l // 2]
        small_b = _bc(small)
        mctx = mod[l, nv - 1]
        cm = [mctx[i * D:(i + 1) * D] for i in range(6)]
        maps = []
        for b in range(B):
            m = mod[l, b]
            sh1, sc1, gt1, sh2, sc2, gt2 = [m[i * D:(i + 1) * D] for i in range(6)]
            vpc = np.stack([_pc(v) for v in (f(g_mix)[l], sc1, sh1, f(g_ffn)[l], sc2, sh2, cm[1], cm[0], cm[4], cm[3], gt2, cm[5])], axis=1)
            vbc = np.stack([_bc(v) for v in (gt1, cm[2], cm[5], f(g_ffn)[l], sc2, sh2, gt2)], axis=1)
            for h in range(NH):
                own = np.arange(h * n_own, (h + 1) * n_own)
                if NH == 2:
                    if h == 0:
                        oth = np.arange(n_own, SEQ)
                    else:
                        oth = np.concatenate([np.arange(n_own - 128, n_own), np.arange(0, n_own - 128)])
                else:
                    oth = np.arange(0, 0)
                d = {
                    "x_own": np.ascontiguousarray(h_lat[b, own]),
                    "x_ctx": np.ascontiguousarray(h_ctx[b]),
                    "w_in": f(w_in)[l], "w_out": f(w_out)[l],
                    "wqup": f(c_w_q_up)[l], "wkvup": f(c_w_kv_up)[l],
                    "vpc": np.ascontiguousarray(vpc), "vbc": np.ascontiguousarray(vbc),
                    "rope": _rope_rows(np.concatenate([own, oth])),
                    "small": small_b, "ident": ident, "masks": _masks(h, NH),
                    "subln_pc": np.ascontiguousarray(f(a_subln)[l].reshape(128, 1)),
                    "ckvn": _bc(f(c_kv_norm)[l]),
                }
                if n_oth:
                    d["x_oth"] = np.ascontiguousarray(h_lat[b, oth])
                if not moe:
                    d["wg"] = f(ffn_w_gate)[l // 2]
                    d["wu"] = f(ffn_w_up)[l // 2]
                    d["wd"] = f(ffn_w_down)[l // 2]
                else:
                    d["wr"] = np.ascontiguousarray(np.broadcast_to(f(moe_w_router)[l // 2].T[None], (128, NEXP, D)))
                maps.append(d)
        r = _run(nc_l, maps)
        if DEBUG:
            DBG[l] = r
            DBG["mod"] = mod
        gfin = _bc(f(g_final))
        if not moe:
            h_lat = np.stack([np.concatenate([r[b * NH + h]["h_out"] for h in range(NH)], axis=0) for b in range(B)], axis=0)
            if not last:
                h_ctx = np.stack([r[b * NH]["hctx_out"] for b in range(B)], axis=0)
            else:
                nc_c = _prog(("comb", n_own, 0), lambda: build_comb(n_own, 0))
                maps = [{"hmid": r[i]["h_out"], "vb": np.ascontiguousarray(np.stack([gfin, gfin], axis=1))} for i in range(B * NH)]
                rc = _run(nc_c, maps)
                out = np.stack([np.concatenate([rc[b * NH + h]["out"] for h in range(NH)], axis=0) for b in range(B)], axis=0)
        else:
            ncore = B * NH
            nt = ncore * n_own
            u2t_all = np.ascontiguousarray(np.concatenate([r[i]["u2t"] for i in range(ncore)], axis=1))
            gates_all = np.concatenate([r[i]["gates"] for i in range(ncore)], axis=0)
            T = 1024
            nc_m = _prog(("moe", nt, T), lambda: build_moe(nt, T))
            maps = []
            for e in range(NEXP):
                maps.append({"u2t": u2t_all,
                             "gate": np.ascontiguousarray(gates_all[:, e].reshape(nt // 128, 128).T),
                             "wg": f(moe_w_gate)[l // 2, e], "wu": f(moe_w_up)[l // 2, e], "wd": f(moe_w_down)[l // 2, e],
                             "ident": ident})
            rm = _run(nc_m, maps)
            nc_c = _prog(("comb", n_own, NEXP), lambda: build_comb(n_own, NEXP))
            maps = []
            for i in range(ncore):
                b = i // NH
                gt2 = mod[l, b][5 * D:6 * D]
                parts = np.ascontiguousarray(np.stack([rm[e]["part"][i * n_own:(i + 1) * n_own] for e in range(NEXP)], axis=0))
                maps.append({"hmid": r[i]["hmid"], "vb": np.ascontiguousarray(np.stack([_bc(gt2), gfin], axis=1)), "parts": parts})
            rc = _run(nc_c, maps)
            assert last
            out = np.stack([np.concatenate([rc[b * NH + h]["out"] for h in range(NH)], axis=0) for b in range(B)], axis=0)
    return out.astype(np.float32)
```
